# Optimizing a Trainium2 kernel written in Bass

```python
import math
import jax, jax.numpy as jnp
from jax import lax
import numpy as np

D_MODEL = 1024
BATCH = 2
SEQ = 8192
DEPTH = 4

Q_BLOCK = 128
NEG_INF = -1e30
LN_EPS = 1e-5
RMS_EPS = 1e-6

MLA_HEADS = 4
MLA_Q_RANK = 256
MLA_KV_RANK = 128
MLA_NOPE = 64
MLA_ROPE = 32
MLA_V = 64
ROPE_THETA = 10000.0

SB_HEADS = 4
SB_DIM = 64

SW_Q_HEADS = 8
SW_KV_HEADS = 2
SW_DIM = 64
SW_WINDOW = 128

DF_HEADS = 4
DF_DIM = 32
DF_VDIM = 2 * DF_DIM

A_WIDTH = MLA_HEADS * MLA_V
B_WIDTH = SB_HEADS * SB_DIM
C_WIDTH = SW_Q_HEADS * SW_DIM
D_WIDTH = DF_HEADS * DF_VDIM
N_BRANCH = 4

IN_SPLITS = (MLA_Q_RANK, MLA_KV_RANK, MLA_ROPE, 3 * B_WIDTH, C_WIDTH, 2 * SW_KV_HEADS * SW_DIM, 2 * DF_HEADS * DF_DIM, 2 * DF_HEADS * DF_DIM, D_WIDTH)
IN_WIDTH = sum(IN_SPLITS)

N_EXPERTS = 32
N_GROUPS = 8
EXPERTS_PER_GROUP = N_EXPERTS // N_GROUPS
GROUP_SCORE_TOP = 2
TOP_K = 2
D_EXPERT = 256
MOE_BLOCK = 256

DN_ALPHA = (2 * DEPTH) ** 0.25
DN_BETA = (8 * DEPTH) ** -0.25

kernel_name = 'hybrid_mla_stickbreak_swa_diff_grouped_moe'


def layer_norm(x, g, b):
    xf = x.astype(jnp.float32)
    mu = xf.mean(-1, keepdims=True)
    var = jnp.square(xf - mu).mean(-1, keepdims=True)
    return ((xf - mu) * lax.rsqrt(var + LN_EPS) * g.astype(jnp.float32) + b.astype(jnp.float32)).astype(x.dtype)


def rms_norm(x, g):
    xf = x.astype(jnp.float32)
    return (xf * lax.rsqrt(jnp.mean(xf * xf, -1, keepdims=True) + RMS_EPS) * g.astype(jnp.float32)).astype(x.dtype)


def apply_rope(t, positions):
    half = t.shape[-1] // 2
    inv_freq = ROPE_THETA ** (-jnp.arange(half, dtype=jnp.float32) / half)
    ang = positions.astype(jnp.float32)[:, None] * inv_freq[None, :]
    cos = jnp.cos(ang)[None, :, None, :]
    sin = jnp.sin(ang)[None, :, None, :]
    tf = t.astype(jnp.float32)
    t1, t2 = tf[..., :half], tf[..., half:]
    return jnp.concatenate([t1 * cos - t2 * sin, t1 * sin + t2 * cos], axis=-1).astype(t.dtype)


def alibi_slopes(n_heads):
    return 2.0 ** (-8.0 * jnp.arange(1, n_heads + 1, dtype=jnp.float32) / n_heads)


def _to_blocks(t):
    b, s = t.shape[:2]
    return t.reshape(b, s // Q_BLOCK, Q_BLOCK, *t.shape[2:]).swapaxes(0, 1)


def _from_blocks(o):
    nq, b, qb = o.shape[:3]
    return o.swapaxes(0, 1).reshape(b, nq * qb, *o.shape[3:])


def mla_attention(q_nope, q_rope, k_nope, k_rope, v):
    s_len = q_nope.shape[1]
    scale = (MLA_NOPE + MLA_ROPE) ** -0.5
    kidx = jnp.arange(s_len)

    def block(args):
        qn, qr, i = args
        qidx = i * Q_BLOCK + jnp.arange(Q_BLOCK)
        sc = (jnp.einsum('bqhd,bkhd->bhqk', qn, k_nope, preferred_element_type=jnp.float32)
              + jnp.einsum('bqhd,bkd->bhqk', qr, k_rope, preferred_element_type=jnp.float32)) * scale
        sc = jnp.where(kidx[None, :] <= qidx[:, None], sc, NEG_INF)
        p = jax.nn.softmax(sc, axis=-1)
        return jnp.einsum('bhqk,bkhd->bqhd', p.astype(v.dtype), v)

    out = lax.map(block, (_to_blocks(q_nope), _to_blocks(q_rope), jnp.arange(s_len // Q_BLOCK)))
    return _from_blocks(out)


def stick_breaking_attention(q, k, v):
    s_len = q.shape[1]
    scale = SB_DIM ** -0.5
    kidx = jnp.arange(s_len)

    def block(args):
        qb, i = args
        qidx = i * Q_BLOCK + jnp.arange(Q_BLOCK)
        z = jnp.einsum('bqhd,bkhd->bhqk', qb, k, preferred_element_type=jnp.float32) * scale
        mask = kidx[None, :] < qidx[:, None]
        log_beta = jax.nn.log_sigmoid(z)
        log_1m_beta = jnp.where(mask, log_beta - z, 0.0)
        suffix = lax.cumsum(log_1m_beta, axis=3, reverse=True) - log_1m_beta
        a = jnp.where(mask, jnp.exp(log_beta + suffix), 0.0)
        return jnp.einsum('bhqk,bkhd->bqhd', a.astype(v.dtype), v)

    out = lax.map(block, (_to_blocks(q), jnp.arange(s_len // Q_BLOCK)))
    return _from_blocks(out)


def sliding_window_attention(q, k, v, sinks, slopes, positions):
    b, s_len, _, d = q.shape
    w = SW_WINDOW
    nb = s_len // w
    g = SW_Q_HEADS // SW_KV_HEADS
    qb = q.reshape(b, nb, w, SW_KV_HEADS, g, d)

    def band(t):
        tb = t.reshape(b, nb, w, SW_KV_HEADS, d)
        prev = jnp.pad(tb[:, :-1], ((0, 0), (1, 0), (0, 0), (0, 0), (0, 0)))
        return jnp.concatenate([prev, tb], axis=2)

    kb, vb = band(k), band(v)
    pq = positions.astype(jnp.float32).reshape(nb, w)
    pk = jnp.concatenate([jnp.pad(pq[:-1], ((1, 0), (0, 0))), pq], axis=1)
    dist = pq[:, :, None] - pk[:, None, :]
    qi = jnp.arange(w)[:, None]
    kj = jnp.arange(2 * w)[None, :]
    offset = qi + w - kj
    in_window = (offset >= 0) & (offset < SW_WINDOW)
    key_exists = (jnp.arange(nb)[:, None, None] > 0) | (kj[None] >= w)
    mask = (in_window[None] & key_exists)[None, :, None, None]
    sc = jnp.einsum('bnqhgd,bnkhd->bnhgqk', qb, kb, preferred_element_type=jnp.float32) * (d ** -0.5)
    bias = -slopes.reshape(SW_KV_HEADS, g)[None, None, :, :, None, None] * dist[None, :, None, None]
    sc = jnp.where(mask, sc + bias, NEG_INF)
    sink = jnp.broadcast_to(sinks.astype(jnp.float32).reshape(SW_KV_HEADS, g)[None, None, :, :, None, None], sc.shape[:-1] + (1,))
    p = jax.nn.softmax(jnp.concatenate([sc, sink], axis=-1), axis=-1)[..., :-1]
    out = jnp.einsum('bnhgqk,bnkhd->bnqhgd', p.astype(v.dtype), vb)
    return out.reshape(b, s_len, SW_Q_HEADS, d)


def diff_attention(q1, q2, k1, k2, v, lam, slopes, positions):
    s_len, d = q1.shape[1], q1.shape[3]
    scale = d ** -0.5
    kidx = jnp.arange(s_len)
    kpos = positions.astype(jnp.float32)

    def block(args):
        a1, a2, i = args
        qidx = i * Q_BLOCK + jnp.arange(Q_BLOCK)
        qpos = lax.dynamic_slice(kpos, (i * Q_BLOCK,), (Q_BLOCK,))
        causal = kidx[None, :] <= qidx[:, None]
        bias = -slopes[:, None, None] * (qpos[:, None] - kpos[None, :])[None]

        def attn_map(a, kk):
            sc = jnp.einsum('bqhd,bkhd->bhqk', a, kk, preferred_element_type=jnp.float32) * scale + bias
            return jax.nn.softmax(jnp.where(causal, sc, NEG_INF), axis=-1)

        p = attn_map(a1, k1) - lam * attn_map(a2, k2)
        return jnp.einsum('bhqk,bkhd->bqhd', p.astype(v.dtype), v)

    out = lax.map(block, (_to_blocks(q1), _to_blocks(q2), jnp.arange(s_len // Q_BLOCK)))
    return _from_blocks(out)


def token_mixers(x, positions, lam_init, w_in, q_norm, kv_norm, w_uq, w_ukv, sinks,
                 lq1, lk1, lq2, lk2, subln, w_gate, b_gate, w_br_a, w_br_b, w_br_c, w_br_d, w_out):
    b, s_len, _ = x.shape
    h = jnp.einsum('bsd,de->bse', x, w_in)
    split_at = [int(c) for c in np.cumsum(IN_SPLITS)[:-1]]
    c_q, c_kv, k_rope_raw, sb_qkv, sw_q, sw_kv, df_q, df_k, df_v = jnp.split(h, split_at, axis=-1)

    q = jnp.einsum('bsr,re->bse', rms_norm(c_q, q_norm), w_uq).reshape(b, s_len, MLA_HEADS, MLA_NOPE + MLA_ROPE)
    q_nope = q[..., :MLA_NOPE]
    q_rope = apply_rope(q[..., MLA_NOPE:], positions)
    kv = jnp.einsum('bsr,re->bse', rms_norm(c_kv, kv_norm), w_ukv).reshape(b, s_len, MLA_HEADS, MLA_NOPE + MLA_V)
    k_nope, v_a = kv[..., :MLA_NOPE], kv[..., MLA_NOPE:]
    k_rope = apply_rope(k_rope_raw[:, :, None, :], positions)[:, :, 0]
    o_a = mla_attention(q_nope, q_rope, k_nope, k_rope, v_a).reshape(b, s_len, A_WIDTH)

    sb = sb_qkv.reshape(b, s_len, 3, SB_HEADS, SB_DIM)
    o_b = stick_breaking_attention(sb[:, :, 0], sb[:, :, 1], sb[:, :, 2]).reshape(b, s_len, B_WIDTH)

    swkv = sw_kv.reshape(b, s_len, 2, SW_KV_HEADS, SW_DIM)
    o_c = sliding_window_attention(sw_q.reshape(b, s_len, SW_Q_HEADS, SW_DIM), swkv[:, :, 0], swkv[:, :, 1],
                                   sinks, alibi_slopes(SW_Q_HEADS), positions).reshape(b, s_len, C_WIDTH)

    dq = df_q.reshape(b, s_len, DF_HEADS, 2, DF_DIM)
    dk = df_k.reshape(b, s_len, DF_HEADS, 2, DF_DIM)
    f32 = jnp.float32
    lam = (jnp.exp(jnp.sum(lq1.astype(f32) * lk1.astype(f32)))
           - jnp.exp(jnp.sum(lq2.astype(f32) * lk2.astype(f32))) + lam_init)
    o = diff_attention(dq[..., 0, :], dq[..., 1, :], dk[..., 0, :], dk[..., 1, :],
                       df_v.reshape(b, s_len, DF_HEADS, DF_VDIM), lam, alibi_slopes(DF_HEADS), positions)
    o_d = (rms_norm(o, subln) * (1.0 - lam_init)).reshape(b, s_len, D_WIDTH)

    gates = jax.nn.sigmoid(jnp.einsum('bsd,de->bse', x, w_gate) + b_gate).reshape(b, s_len, N_BRANCH, D_MODEL)
    merged = (gates[:, :, 0] * jnp.einsum('bsw,wd->bsd', o_a, w_br_a)
              + gates[:, :, 1] * jnp.einsum('bsw,wd->bsd', o_b, w_br_b)
              + gates[:, :, 2] * jnp.einsum('bsw,wd->bsd', o_c, w_br_c)
              + gates[:, :, 3] * jnp.einsum('bsw,wd->bsd', o_d, w_br_d))
    return jnp.einsum('bsd,de->bse', merged, w_out)


def grouped_moe(x, w_router, router_bias, w1, w3, w2):
    b, s_len, d = x.shape
    xt = x.reshape(b * s_len, d)
    t = xt.shape[0]
    scores = jax.nn.sigmoid(jnp.dot(xt.astype(jnp.float32), w_router.astype(jnp.float32)))
    biased = (scores + router_bias.astype(jnp.float32)).reshape(t, N_GROUPS, EXPERTS_PER_GROUP)
    group_score = lax.top_k(biased, GROUP_SCORE_TOP)[0].sum(-1)
    g_sel = jnp.argmax(group_score, axis=-1).astype(jnp.int32)
    in_group = jnp.take_along_axis(biased, g_sel[:, None, None], axis=1)[:, 0]
    local = lax.top_k(in_group, TOP_K)[1]
    e_idx = (g_sel[:, None] * EXPERTS_PER_GROUP + local).astype(jnp.int32)
    gate = jnp.take_along_axis(scores, e_idx, axis=1)
    gate = gate / gate.sum(-1, keepdims=True)

    tk = t * TOP_K
    flat_e = e_idx.reshape(-1)
    order = jnp.argsort(flat_e)
    sorted_e = flat_e[order]
    tok = (order // TOP_K).astype(jnp.int32)
    counts = jnp.bincount(flat_e, length=N_EXPERTS)
    padded = (counts + MOE_BLOCK - 1) // MOE_BLOCK * MOE_BLOCK
    pad_end = jnp.cumsum(padded)
    pad_start = pad_end - padded
    start = jnp.cumsum(counts) - counts
    dest = (pad_start[sorted_e] + jnp.arange(tk) - start[sorted_e]).astype(jnp.int32)
    n_blocks = -(-tk // MOE_BLOCK) + N_EXPERTS
    rows = n_blocks * MOE_BLOCK
    row_tok = jnp.full((rows,), t, jnp.int32).at[dest].set(tok)
    x_pad = jnp.concatenate([xt, jnp.zeros((1, d), xt.dtype)], axis=0)
    xs = x_pad[row_tok].reshape(n_blocks, MOE_BLOCK, d)
    block_e = jnp.minimum(jnp.searchsorted(pad_end, jnp.arange(n_blocks) * MOE_BLOCK, side='right'), N_EXPERTS - 1)

    def expert_block(args):
        xb, e = args
        hid = jax.nn.silu(xb @ w1[e]) * (xb @ w3[e])
        return hid @ w2[e]

    ys = lax.map(expert_block, (xs, block_e)).reshape(rows, d)
    gate_sorted = gate.reshape(-1)[order].astype(x.dtype)
    y = jnp.zeros((t, d), x.dtype).at[tok].add(ys[dest] * gate_sorted[:, None])
    return y.reshape(b, s_len, d)


def setup_inputs(seed: int = 0) -> dict:
    key = jax.random.key(seed)
    ks = iter(jax.random.split(key, 40))

    def nrm(shape, scale):
        return jax.random.normal(next(ks), shape, jnp.float32) * scale

    def gain(shape):
        return 1.0 + 0.02 * jax.random.normal(next(ks), shape, jnp.float32)

    L = DEPTH
    return {
        'x': nrm((BATCH, SEQ, D_MODEL), 1.0),
        'positions': jnp.arange(SEQ, dtype=jnp.int32),
        'w_in': nrm((L, D_MODEL, IN_WIDTH), D_MODEL ** -0.5),
        'mla_q_norm': gain((L, MLA_Q_RANK)),
        'mla_kv_norm': gain((L, MLA_KV_RANK)),
        'w_uq': nrm((L, MLA_Q_RANK, MLA_HEADS * (MLA_NOPE + MLA_ROPE)), MLA_Q_RANK ** -0.5),
        'w_ukv': nrm((L, MLA_KV_RANK, MLA_HEADS * (MLA_NOPE + MLA_V)), MLA_KV_RANK ** -0.5),
        'sw_sinks': nrm((L, SW_Q_HEADS), 0.5),
        'df_lq1': nrm((L, DF_DIM), 0.1),
        'df_lk1': nrm((L, DF_DIM), 0.1),
        'df_lq2': nrm((L, DF_DIM), 0.1),
        'df_lk2': nrm((L, DF_DIM), 0.1),
        'df_subln': gain((L, DF_VDIM)),
        'w_gate': nrm((L, D_MODEL, N_BRANCH * D_MODEL), D_MODEL ** -0.5),
        'b_gate': nrm((L, N_BRANCH * D_MODEL), 0.01),
        'w_br_a': nrm((L, A_WIDTH, D_MODEL), DN_BETA * A_WIDTH ** -0.5),
        'w_br_b': nrm((L, B_WIDTH, D_MODEL), DN_BETA * B_WIDTH ** -0.5),
        'w_br_c': nrm((L, C_WIDTH, D_MODEL), DN_BETA * C_WIDTH ** -0.5),
        'w_br_d': nrm((L, D_WIDTH, D_MODEL), DN_BETA * D_WIDTH ** -0.5),
        'w_out': nrm((L, D_MODEL, D_MODEL), DN_BETA * D_MODEL ** -0.5),
        'ln1_g': gain((L, D_MODEL)),
        'ln1_b': nrm((L, D_MODEL), 0.01),
        'w_router': nrm((D_MODEL, N_EXPERTS), D_MODEL ** -0.5),
        'router_bias': nrm((N_EXPERTS,), 0.01),
        'moe_w1': nrm((L, N_EXPERTS, D_MODEL, D_EXPERT), D_MODEL ** -0.5),
        'moe_w3': nrm((L, N_EXPERTS, D_MODEL, D_EXPERT), DN_BETA * D_MODEL ** -0.5),
        'moe_w2': nrm((L, N_EXPERTS, D_EXPERT, D_MODEL), DN_BETA * D_EXPERT ** -0.5),
        'ln2_g': gain((L, D_MODEL)),
        'ln2_b': nrm((L, D_MODEL), 0.01),
    }


def reference(x, positions, w_in, mla_q_norm, mla_kv_norm, w_uq, w_ukv, sw_sinks,
              df_lq1, df_lk1, df_lq2, df_lk2, df_subln, w_gate, b_gate,
              w_br_a, w_br_b, w_br_c, w_br_d, w_out, ln1_g, ln1_b,
              w_router, router_bias, moe_w1, moe_w3, moe_w2, ln2_g, ln2_b):
    for l in range(DEPTH):
        lam_init = 0.8 - 0.6 * math.exp(-0.3 * l)
        mix = token_mixers(x, positions, lam_init, w_in[l], mla_q_norm[l], mla_kv_norm[l], w_uq[l], w_ukv[l],
                           sw_sinks[l], df_lq1[l], df_lk1[l], df_lq2[l], df_lk2[l], df_subln[l],
                           w_gate[l], b_gate[l], w_br_a[l], w_br_b[l], w_br_c[l], w_br_d[l], w_out[l])
        x = layer_norm(DN_ALPHA * x + mix, ln1_g[l], ln1_b[l])
        ffn = grouped_moe(x, w_router, router_bias, moe_w1[l], moe_w3[l], moe_w2[l])
        x = layer_norm(DN_ALPHA * x + ffn, ln2_g[l], ln2_b[l])
    return x
```

```python
import math
from concourse.bass_utils import run_bass_kernel_spmd
import numpy as np
from contextlib import ExitStack
import concourse.bass as bass
import concourse.mybir as mybir

F32 = mybir.dt.float32
BF16 = mybir.dt.bfloat16
I32 = mybir.dt.int32
AF = mybir.ActivationFunctionType
ALU = mybir.AluOpType
AX = mybir.AxisListType


class Prog:
    ENGS = ("pe", "act", "dve", "pool", "sp")

    def __init__(self, nc):
        self.nc = nc
        self.ops = {e: [] for e in self.ENGS}
        self.cnt = {e: 0 for e in self.ENGS}
        self.dcnt = {}
        self.waited = {}
        self.lastw = {}
        self.readers = {}
        self.stack = ExitStack()
        self.nsb = 0
        self.excl = set()

    def sb(self, shape, dt, name=None):
        self.nsb += 1
        return self.stack.enter_context(self.nc.sbuf_tensor((name + "_sbuf") if name else f"sb{self.nsb}", list(shape), dt))

    def ps(self, shape, dt=F32, name=None):
        self.nsb += 1
        return self.stack.enter_context(self.nc.psum_tensor(name or f"ps{self.nsb}", list(shape), dt))

    def _deps(self, eng, reads, writes):
        deps = set()
        for r in reads:
            if r in self.lastw:
                deps.add(self.lastw[r])
            if r in self.excl:
                for t in self.readers.get(r, ()):
                    if not (t[0] == "eng" and t[1] == eng):
                        deps.add(t)
        for w in writes:
            if w in self.lastw:
                deps.add(self.lastw[w])
            for t in self.readers.get(w, ()):
                deps.add(t)
        best = {}
        for (kind, key, val) in deps:
            if kind == "eng" and key == eng and eng in ("pe", "sp"):
                continue
            k = (kind, key)
            if val > best.get(k, 0):
                best[k] = val
        waits = []
        for k, val in best.items():
            if val > self.waited.get((eng, k), 0):
                self.waited[(eng, k)] = val
                waits.append((k, val))
        return waits

    def _mark(self, tok, reads, writes):
        for w in writes:
            self.lastw[w] = tok
            self.readers[w] = []
        for r in reads:
            self.readers.setdefault(r, []).append(tok)

    def op(self, eng, fn, reads=(), writes=()):
        waits = self._deps(eng, reads, writes)
        self.cnt[eng] += 1
        tok = ("eng", eng, self.cnt[eng])
        self.ops[eng].append((waits, fn, ("eng", eng)))
        self._mark(tok, reads, writes)
        return tok

    def dma(self, q, out, in_, sem, reads=(), writes=()):
        waits = self._deps(q, reads, writes)
        self.dcnt[sem] = self.dcnt.get(sem, 0) + 16
        tok = ("dma", sem, self.dcnt[sem])
        self.ops[q].append((waits, lambda e: e.dma_start(out=out, in_=in_), ("dma", sem)))
        self._mark(tok, reads, writes)
        return tok

    def mm(self, out, lhsT, rhs, start=True, stop=True, reads=(), writes=()):
        return self.op("pe", lambda e: e.matmul(out, lhsT, rhs, start=start, stop=stop), reads, writes)

    def tr(self, out, in_, ident, reads=(), writes=()):
        return self.op("pe", lambda e: e.transpose(out, in_, ident), reads, writes)

    def act(self, out, in_, func, bias=None, scale=None, accum_out=None, reads=(), writes=(), eng="act"):
        kw = {}
        if bias is not None:
            kw["bias"] = bias
        if scale is not None:
            kw["scale"] = scale
        if accum_out is not None:
            kw["accum_out"] = accum_out
        return self.op(eng, lambda e: e.activation(out, in_, func, **kw), reads, writes)

    def ts(self, out, in0, s1, s2, op0, op1=None, reads=(), writes=(), eng="dve", accum_out=None):
        kw = {}
        if op1 is not None:
            kw["op1"] = op1
        if accum_out is not None:
            kw["accum_out"] = accum_out
        return self.op(eng, lambda e: e.tensor_scalar(out, in0, s1, s2, op0, **kw), reads, writes)

    def tt(self, out, in0, in1, op, reads=(), writes=(), eng="dve"):
        return self.op(eng, lambda e: e.tensor_tensor(out, in0, in1, op), reads, writes)

    def stt(self, out, in0, scalar, in1, op0, op1, reads=(), writes=(), eng="dve"):
        return self.op(eng, lambda e: e.scalar_tensor_tensor(out, in0, scalar, in1, op0, op1), reads, writes)

    def cp(self, out, in_, reads=(), writes=(), eng="dve"):
        if eng == "act":
            return self.op(eng, lambda e: e.activation(out, in_, AF.Copy), reads, writes)
        return self.op(eng, lambda e: e.tensor_copy(out, in_), reads, writes)

    def recip(self, out, in_, reads=(), writes=(), eng="dve"):
        return self.op(eng, lambda e: e.reciprocal(out, in_), reads, writes)

    def memset(self, out, val, writes=(), eng="pool"):
        return self.op(eng, lambda e: e.memset(out, val), (), writes)

    def emit(self):
        nc = self.nc
        sems = {}
        for e in self.ENGS:
            sems[("eng", e)] = self.stack.enter_context(nc.semaphore("s_" + e))
        for s in self.dcnt:
            sems[("dma", s)] = self.stack.enter_context(nc.semaphore("d_" + s))
        final = [(("eng", e), self.cnt[e]) for e in self.ENGS if self.cnt[e] > 0 and e != "sp"]
        final += [(("dma", s), v) for s, v in self.dcnt.items()]
        ops = self.ops

        def run(e, name):
            for waits, fn, semk in ops[name]:
                for k, val in waits:
                    e.wait_ge(sems[k], val)
                ins = fn(e)
                if semk[0] == "dma":
                    ins.then_inc(sems[semk], 16)
                else:
                    ins.then_inc(sems[semk], 1)

        with nc.Block() as block:
            @block.tensor
            def _(e):
                run(e, "pe")

            @block.scalar
            def _(e):
                run(e, "act")

            @block.vector
            def _(e):
                run(e, "dve")

            @block.gpsimd
            def _(e):
                run(e, "pool")

            @block.sync
            def _(e):
                run(e, "sp")
                for k, val in final:
                    e.wait_ge(sems[k], val)
        self.stack.close()
        return nc

LN_EPS = 1e-5
RMS_EPS = 1e-6
MAGIC = 12582912.0
C1 = 6.28125
C2 = 2.0 * math.pi - 6.28125
G = {}
_off = 0
for _n, _m in (("cq0", 128), ("cq1", 128), ("ckv", 128), ("kr", 96), ("krs", 96), ("qB", 96), ("kB", 96),
               ("qC", 32), ("kC", 32), ("swqA", 64), ("swqB", 64), ("swk", 64)):
    G[_n] = (_off, _m)
    _off += _m
NT = _off


def pipeline(n, stages):
    mx = max(o for o, _ in stages)
    for t in range(-mx, n):
        for off, fn in stages:
            i = t + off
            if 0 <= i < n:
                fn(i)


def build_phaseA(S):
    nc = bass.Bass("TRN2", target_bir_lowering=False)
    P = Prog(nc)
    NCH = S // 512
    NB = S // 128

    def din(name, shape, dt=F32):
        return nc.dram_tensor(name, list(shape), dt, kind="ExternalInput").ap()

    xT_d = din("xT", [1024, S])
    wT_d = din("wT", [1024, NT])
    wV_d = din("wV", [1024, 192])
    wuq_d = din("wuq", [256, 192])
    wukv_d = din("wukv", [128, 128])
    qn_d = din("qn", [128, 2])
    kvn_d = din("kvn", [128, 1])
    cst_d = din("cst", [128, 16])
    lqk_d = din("lqk", [64, 128])
    posK_d = din("posK", [128, NB], I32)
    pmid_d = din("pmidB", [128, NCH], I32)
    ref_d = din("refB", [128, NB], I32)
    pos32_d = din("pos32", [32, S], I32)
    posrow_d = din("posrow", [1, S], I32)
    refrow_d = din("refrow", [1, S], I32)
    oT_d = nc.dram_tensor("oT", [320, S], BF16, kind="ExternalOutput").ap()

    wT = P.sb([128, 8, NT], BF16, "wT_sb")
    wV = P.sb([128, 8, 192], BF16, "wV_sb")
    wuq = P.sb([128, 2, 192], BF16, "wuq_sb")
    wukv = P.sb([128, 128], BF16, "wukv_sb")
    stg = [P.sb([128, 1024], F32, f"stg{i}") for i in range(2)]
    qn = P.sb([128, 2], F32, "qn_sb")
    kvn = P.sb([128, 1], F32, "kvn_sb")
    cst = P.sb([128, 16], F32, "cst_sb")
    lqk = P.sb([64, 128], F32, "lqk_sb")
    small = P.sb([128, 16], F32, "small")
    posKi = P.sb([128, NB], I32, "posKi")
    posKf = P.sb([128, NB], F32, "posKf")
    pmidi = P.sb([128, NCH], I32, "pmidi")
    pmidf = P.sb([128, NCH], F32, "pmidf")
    refi = P.sb([128, NB], I32, "refi")
    reff = P.sb([128, NB], F32, "reff")
    bsw = [[P.sb([128, NB], F32, f"bsw{h}{w}") for w in range(2)] for h in range(2)]
    btab = P.sb([128, NB], F32, "btab")
    onesb = P.sb([128, 128], BF16, "onesb")
    onesf = P.sb([128, 64], F32, "onesf")
    Um = P.sb([128, 128], BF16, "Um")
    On8 = P.sb([128, 128], BF16, "On8")
    ioi = P.sb([128, 512], I32, "ioi")
    iof = P.sb([128, 512], F32, "iof")
    mI = [P.sb([128, 512], BF16, f"mI{o}") for o in range(4)]
    mS = [P.sb([128, 512], BF16, f"mS{o}") for o in range(4)]
    mSW = [P.sb([128, 256], BF16, f"mSW{w}") for w in range(2)]
    KA = P.sb([128, S], BF16, "KA")
    KB = P.sb([128, S], BF16, "KB")
    KC = P.sb([32, S], BF16, "KC")
    KSW = P.sb([64, 2, 512], BF16, "KSW")
    Va = P.sb([128, NB, 65], BF16, "Va")
    Vsb = P.sb([128, NB, 65], BF16, "Vsb")
    Vdf = P.sb([128, NB, 65], BF16, "Vdf")
    Vsw = P.sb([128, 8, 65], BF16, "Vsw")
    xs = [P.sb([128, 512], F32, f"xs{i}") for i in range(2)]
    xbf = [P.sb([128, 8, 512], BF16, f"xbf{i}") for i in range(2)]
    QA = [P.sb([96, 512], BF16, f"QA{i}") for i in range(2)]
    QB = [P.sb([96, 512], BF16, f"QB{i}") for i in range(2)]
    QC = [P.sb([32, 512], BF16, f"QC{i}") for i in range(2)]
    QSW = [P.sb([64, 2, 512], BF16, f"QSW{i}") for i in range(2)]
    cqb = P.sb([128, 2, 512], BF16, "cqb")
    sqq = P.sb([128, 2, 512], BF16, "sqq")
    ckvb = P.sb([128, 512], BF16, "ckvb")
    sqkv = P.sb([128, 512], BF16, "sqkv")
    rq = P.sb([128, 512], F32, "rq")
    rkv = P.sb([128, 512], F32, "rkv")
    rp_i = P.sb([96, 512], I32, "rp_i")
    rp_a = P.sb([96, 512], F32, "rp_a")
    rp_b = P.sb([96, 512], F32, "rp_b")
    cosT = P.sb([96, 512], F32, "cosT")
    sinT = P.sb([96, 512], F32, "sinT")
    t1 = P.sb([96, 512], F32, "t1")
    t2 = P.sb([96, 512], F32, "t2")
    rowi = P.sb([65, 512], I32, "rowi")
    rowi2 = P.sb([65, 512], I32, "rowi2")
    rowf = P.sb([65, 512], F32, "rowf")
    sinkrow = [P.sb([65, 512], F32, f"sinkrow{h}") for h in range(2)]
    Pt = [P.sb([128, 512], BF16, f"Pt{i}") for i in range(3)]
    ef = [P.sb([128, 512], F32, "ef0")]
    spb = [P.sb([128, 512], BF16, f"spb{i}") for i in range(2)]
    Rb = P.sb([128, 512], BF16, "Rb")
    dn = P.sb([65, 512], F32, "dn")
    bc = P.sb([64, 512], F32, "bc")
    fa = P.sb([64, 512], F32, "fa")
    fb = P.sb([64, 512], F32, "fb")
    fsq = P.sb([64, 512], BF16, "fsq")
    ob = [P.sb([64, 512], BF16, f"ob{i}") for i in range(3)]
    rstok = P.sb([128, 4], F32, "rstok")
    B = [P.ps([128, 512], F32, f"bank{i}") for i in range(8)]
    P.excl = set(f"B{i}" for i in range(8))

    P.dma("sp", cst[:], cst_d[:, :], "c0", writes=["cst"])
    P.dma("sp", qn[:], qn_d[:, :], "c1", writes=["qn"])
    P.dma("sp", kvn[:], kvn_d[:, :], "c2", writes=["kvn"])
    P.dma("sp", lqk[:], lqk_d[:, :], "c3", writes=["lqk"])
    P.dma("sp", posKi[:], posK_d[:, :], "c4", writes=["posKi"])
    P.dma("sp", pmidi[:], pmid_d[:, :], "c5", writes=["pmidi"])
    P.dma("sp", refi[:], ref_d[:, :], "c6", writes=["refi"])
    P.cp(posKf[:], posKi[:], reads=["posKi"], writes=["posKf"])
    P.cp(pmidf[:], pmidi[:], reads=["pmidi"], writes=["pmidf"])
    P.cp(reff[:], refi[:], reads=["refi"], writes=["reff"])
    P.memset(onesb[:], 1.0, writes=["onesb"])
    P.memset(onesf[:], 1.0, writes=["onesf"])
    P.memset(On8[:], -8.0, writes=["On8"])
    P.memset(Va[:, :, 64:65], 1.0, writes=["Va1"])
    P.memset(Vsb[:, :, 64:65], 1.0, writes=["Vsb1"])
    P.memset(Vdf[:, :, 64:65], 1.0, writes=["Vdf1"])
    P.memset(Vsw[:, :, 64:65], 1.0, writes=["Vsw1"])
    P.op("pool", lambda e: e.iota(ioi[:], [[1, 512]], base=0, channel_multiplier=-1), writes=["ioi"])
    P.cp(iof[:], ioi[:], reads=["ioi"], writes=["iof"])
    for o in range(4):
        P.ts(mI[o][:], iof[:], float(128 * o), None, ALU.is_ge, reads=["iof"], writes=[f"mI{o}"])
        P.ts(mS[o][:], iof[:], float(128 * o + 1), None, ALU.is_ge, reads=["iof"], writes=[f"mS{o}"])
    for hh in range(2):
        P.ts(mSW[0][:, hh * 128:(hh + 1) * 128], iof[:, 0:128], -1.0, None, ALU.is_le, reads=["iof"], writes=["mSW0"])
        P.ts(mSW[1][:, hh * 128:(hh + 1) * 128], iof[:, 0:128], 0.0, None, ALU.is_ge, reads=["iof"], writes=["mSW1"])
    P.ts(Um[:], iof[:, 0:128], 0.0, -8.0, ALU.is_le, op1=ALU.mult, reads=["iof"], writes=["Um"])
    for hh in range(2):
        sl = cst[:, 3 + hh:4 + hh]
        if NB > 1:
            P.tt(bsw[hh][0][:, 1:NB], posKf[:, 0:NB - 1], reff[:, 1:NB], ALU.subtract, reads=["posKf", "reff"], writes=[f"bsw{hh}0"])
            P.ts(bsw[hh][0][:, 1:NB], bsw[hh][0][:, 1:NB], sl, None, ALU.mult, reads=[f"bsw{hh}0", "cst"], writes=[f"bsw{hh}0"])
        P.tt(bsw[hh][1][:], posKf[:], reff[:], ALU.subtract, reads=["posKf", "reff"], writes=[f"bsw{hh}1"])
        P.ts(bsw[hh][1][:], bsw[hh][1][:], sl, None, ALU.mult, reads=[f"bsw{hh}1", "cst"], writes=[f"bsw{hh}1"])
    P.tt(t1[0:64, 0:32], lqk[0:64, 0:32], lqk[0:64, 32:64], ALU.mult, reads=["lqk"], writes=["t1"])
    P.op("dve", lambda e: e.tensor_reduce(small[0:64, 0:1], t1[0:64, 0:32], AX.X, ALU.add), reads=["t1"], writes=["small"])
    P.tt(t1[0:64, 0:32], lqk[0:64, 64:96], lqk[0:64, 96:128], ALU.mult, reads=["lqk", "t1", "small"], writes=["t1"])
    P.op("dve", lambda e: e.tensor_reduce(small[0:64, 1:2], t1[0:64, 0:32], AX.X, ALU.add), reads=["t1"], writes=["small"])
    P.act(small[0:64, 2:4], small[0:64, 0:2], AF.Exp, reads=["small"], writes=["small"])
    P.tt(small[0:64, 4:5], small[0:64, 3:4], small[0:64, 2:3], ALU.subtract, reads=["small"], writes=["small"])
    P.tt(small[0:64, 5:6], small[0:64, 4:5], cst[0:64, 7:8], ALU.subtract, reads=["small", "cst"], writes=["small"])
    neglam = small[0:64, 5:6]

    si = [0]

    def load_cast(dst, src, rows, cols, scale_ap=None, key=None):
        i = si[0] % 2
        si[0] += 1
        P.dma("sp", stg[i][0:rows, 0:cols], src, f"stg{i}", writes=[f"stg{i}"])
        if scale_ap is None:
            P.cp(dst, stg[i][0:rows, 0:cols], reads=[f"stg{i}"], writes=[key], eng="pool" if si[0] % 2 else "dve")
        else:
            P.ts(dst, stg[i][0:rows, 0:cols], scale_ap, None, ALU.mult, reads=[f"stg{i}", "qn", "kvn"], writes=[key])

    for dc in range(8):
        load_cast(wT[:, dc, :], wT_d[dc * 128:(dc + 1) * 128, :], 128, NT, key="wT")
        load_cast(wV[:, dc, :], wV_d[dc * 128:(dc + 1) * 128, :], 128, 192, key="wV")
    for rc in range(2):
        load_cast(wuq[:, rc, :], wuq_d[rc * 128:(rc + 1) * 128, :], 128, 192, scale_ap=qn[:, rc:rc + 1], key="wuq")
    load_cast(wukv[:, :], wukv_d[:, :], 128, 128, scale_ap=kvn[:, 0:1], key="wukv")

    xcnt = [0]

    for c in range(NCH):
        t0 = c * 512
        cb = c % 2
        X = xbf[cb]
        xk = f"xbf{cb}"
        for dc in range(8):
            i = xcnt[0] % 2
            xcnt[0] += 1
            P.dma("sp", xs[i][:], xT_d[dc * 128:(dc + 1) * 128, t0:t0 + 512], f"xs{i}", writes=[f"xs{i}"])
            P.cp(X[:, dc, :], xs[i][:], reads=[f"xs{i}"], writes=[xk], eng="pool" if dc % 2 else "dve")
        P.dma("sp", rp_i[64:96, :], pos32_d[:, t0:t0 + 512], "rp", writes=["rp_i"])
        P.cp(rp_a[64:96, :], rp_i[64:96, :], reads=["rp_i"], writes=["rp_a"])
        P.ts(rp_a[64:96, :], rp_a[64:96, :], cst[64:96, 0:1], None, ALU.mult, reads=["rp_a", "cst"], writes=["rp_a"])
        P.ts(rp_b[64:96, :], rp_a[64:96, :], 1.0 / (2 * math.pi), MAGIC, ALU.mult, op1=ALU.add, reads=["rp_a"], writes=["rp_b"])
        P.ts(rp_b[64:96, :], rp_b[64:96, :], MAGIC, None, ALU.subtract, reads=["rp_b"], writes=["rp_b"])
        P.stt(rp_a[64:96, :], rp_b[64:96, :], -C1, rp_a[64:96, :], ALU.mult, ALU.add, reads=["rp_a", "rp_b"], writes=["rp_a"])
        P.stt(rp_a[64:96, :], rp_b[64:96, :], -C2, rp_a[64:96, :], ALU.mult, ALU.add, reads=["rp_a", "rp_b"], writes=["rp_a"])
        P.ts(rp_a[64:96, :], rp_a[64:96, :], math.pi, -math.pi, ALU.min, op1=ALU.max, reads=["rp_a"], writes=["rp_a"])
        P.act(sinT[64:96, :], rp_a[64:96, :], AF.Sin, scale=cst[64:96, 1:2], reads=["rp_a", "cst"], writes=["sinT"])
        P.act(rp_b[64:96, :], rp_a[64:96, :], AF.Abs, reads=["rp_a", "rp_b"], writes=["rp_b"])
        P.act(cosT[64:96, :], rp_b[64:96, :], AF.Sin, scale=-1.0, bias=cst[64:96, 11:12], reads=["rp_b", "cst"], writes=["cosT"])
        P.dma("sp", rowi[64:65, :], posrow_d[:, t0:t0 + 512], "rw1", writes=["rowi"])
        P.dma("sp", rowi2[64:65, :], refrow_d[:, t0:t0 + 512], "rw2", writes=["rowi2"])
        P.tt(rowf[64:65, :], rowi[64:65, :], rowi2[64:65, :], ALU.subtract, reads=["rowi", "rowi2"], writes=["rowf"])
        for hh in range(2):
            P.act(sinkrow[hh][64:65, :], rowf[64:65, :], AF.Exp, scale=cst[64:65, 3 + hh:4 + hh], bias=cst[64:65, 9 + hh:10 + hh],
                  reads=["rowf", "cst"], writes=[f"sinkrow{hh}"])

        def proj(gname, bank):
            off, M = G[gname]
            for dc in range(8):
                P.mm(B[bank][0:M, :], wT[:, dc, off:off + M], X[:, dc, :], start=(dc == 0), stop=(dc == 7),
                     reads=["wT", xk], writes=[f"B{bank}"])
            return M

        proj("cq0", 0)
        P.cp(cqb[:, 0, :], B[0][:, :], reads=["B0"], writes=["cqb"])
        P.act(sqq[:, 0, :], B[0][:, :], AF.Square, reads=["B0"], writes=["sqq"])
        proj("cq1", 1)
        P.cp(cqb[:, 1, :], B[1][:, :], reads=["B1"], writes=["cqb"])
        P.act(sqq[:, 1, :], B[1][:, :], AF.Square, reads=["B1"], writes=["sqq"])
        proj("ckv", 2)
        P.cp(ckvb[:, :], B[2][:, :], reads=["B2"], writes=["ckvb"])
        P.act(sqkv[:, :], B[2][:, :], AF.Square, reads=["B2"], writes=["sqkv"])
        P.mm(B[6][:, :], onesb[:, :], sqq[:, 0, :], start=True, stop=False, reads=["onesb", "sqq"], writes=["B6"])
        P.mm(B[6][:, :], onesb[:, :], sqq[:, 1, :], start=False, stop=True, reads=["onesb", "sqq"], writes=["B6"])
        P.act(rq[:], B[6][:, :], AF.Sqrt, scale=1.0 / 256, bias=cst[:, 12:13], reads=["B6", "cst"], writes=["rq"])
        P.recip(rq[:], rq[:], reads=["rq"], writes=["rq"])
        P.mm(B[7][:, :], onesb[:, :], sqkv[:, :], reads=["onesb", "sqkv"], writes=["B7"])
        P.act(rkv[:], B[7][:, :], AF.Sqrt, scale=1.0 / 128, bias=cst[:, 12:13], reads=["B7", "cst"], writes=["rkv"])
        P.recip(rkv[:], rkv[:], reads=["rkv"], writes=["rkv"])
        proj("kr", 3)
        proj("krs", 4)
        P.tt(t1[64:96, :], B[3][64:96, :], cosT[64:96, :], ALU.mult, reads=["B3", "cosT"], writes=["t1"])
        P.tt(t2[64:96, :], B[4][64:96, :], sinT[64:96, :], ALU.mult, reads=["B4", "sinT"], writes=["t2"])
        P.tt(KA[64:96, t0:t0 + 512], t1[64:96, :], t2[64:96, :], ALU.add, reads=["t1", "t2"], writes=[f"KA{c}"])
        for rc in range(2):
            P.mm(B[0][0:96, :], wuq[:, rc, 0:96], cqb[:, rc, :], start=(rc == 0), stop=(rc == 1), reads=["wuq", "cqb"], writes=["B0"])
        for rc in range(2):
            P.mm(B[1][0:96, :], wuq[:, rc, 96:192], cqb[:, rc, :], start=(rc == 0), stop=(rc == 1), reads=["wuq", "cqb"], writes=["B1"])
        qak = f"QA{cb}"
        P.tt(QA[cb][0:64, :], B[0][0:64, :], rq[0:64, :], ALU.mult, reads=["B0", "rq"], writes=[qak])
        P.tt(t1[64:96, :], B[0][64:96, :], cosT[64:96, :], ALU.mult, reads=["B0", "cosT", "t1"], writes=["t1"])
        P.tt(t2[64:96, :], B[1][64:96, :], sinT[64:96, :], ALU.mult, reads=["B1", "sinT", "t2"], writes=["t2"])
        P.tt(t1[64:96, :], t1[64:96, :], t2[64:96, :], ALU.add, reads=["t1", "t2"], writes=["t1"])
        P.tt(QA[cb][64:96, :], t1[64:96, :], rq[64:96, :], ALU.mult, reads=["t1", "rq"], writes=[qak])
        P.mm(B[2][0:64, :], wukv[:, 0:64], ckvb[:, :], reads=["wukv", "ckvb"], writes=["B2"])
        P.tt(KA[0:64, t0:t0 + 512], B[2][0:64, :], rkv[0:64, :], ALU.mult, reads=["B2", "rkv"], writes=[f"KA{c}"])
        proj("qB", 3)
        P.cp(QB[cb][0:96, :], B[3][0:96, :], reads=["B3"], writes=[f"QB{cb}"], eng="act")
        proj("kB", 4)
        P.cp(KB[0:96, t0:t0 + 512], B[4][0:96, :], reads=["B4"], writes=[f"KB{c}"])
        proj("qC", 0)
        P.cp(QC[cb][0:32, :], B[0][0:32, :], reads=["B0"], writes=[f"QC{cb}"], eng="act")
        proj("kC", 1)
        P.cp(KC[0:32, t0:t0 + 512], B[1][0:32, :], reads=["B1"], writes=[f"KC{c}"])
        proj("swqA", 2)
        P.cp(QSW[cb][0:64, 0, :], B[2][0:64, :], reads=["B2"], writes=[f"QSW{cb}"], eng="act")
        proj("swqB", 3)
        P.cp(QSW[cb][0:64, 1, :], B[3][0:64, :], reads=["B3"], writes=[f"QSW{cb}"])
        proj("swk", 4)
        P.cp(KSW[0:64, cb, :], B[4][0:64, :], reads=["B4"], writes=[f"KSW{cb}"], eng="act")
        for j in range(4):
            blk = 4 * c + j
            for dc in range(8):
                P.mm(B[5][:, 0:192], X[:, dc, j * 128:(j + 1) * 128], wV[:, dc, :], start=(dc == 0), stop=(dc == 7),
                     reads=[xk, "wV"], writes=["B5"])
            P.cp(Vsb[:, blk, 0:64], B[5][:, 0:64], reads=["B5"], writes=[f"Vsb{blk}"])
            P.cp(Vsw[:, blk % 8, 0:64], B[5][:, 64:128], reads=["B5"], writes=[f"Vsw{blk % 8}"], eng="act")
            P.cp(Vdf[:, blk, 0:64], B[5][:, 128:192], reads=["B5"], writes=[f"Vdf{blk}"])
            P.mm(B[6][:, 0:64], ckvb[:, j * 128:(j + 1) * 128], wukv[:, 64:128], reads=["ckvb", "wukv"], writes=["B6"])
            P.mm(B[7][:, 0:1], sqkv[:, j * 128:(j + 1) * 128], onesb[:, 0:1], reads=["sqkv", "onesb"], writes=["B7"])
            P.act(rstok[:, j:j + 1], B[7][:, 0:1], AF.Sqrt, scale=1.0 / 128, bias=cst[:, 12:13], reads=["B7", "cst"], writes=["rstok"])
            P.recip(rstok[:, j:j + 1], rstok[:, j:j + 1], reads=["rstok"], writes=["rstok"])
            P.ts(Va[:, blk, 0:64], B[6][:, 0:64], rstok[:, j:j + 1], None, ALU.mult, reads=["B6", "rstok"], writes=[f"Va{blk}"])

        nkb = 4 * c + 4
        oi = [0]

        def finalize_norm(obank, rows_out, extra_den=None, post=None):
            bk = f"B{obank}"
            if extra_den is None:
                P.cp(dn[64:65, :], B[obank][64:65, :], reads=[bk], writes=["dn"])
            else:
                ap, key = extra_den
                P.tt(dn[64:65, :], B[obank][64:65, :], ap, ALU.add, reads=[bk, key], writes=["dn"])
            P.recip(dn[64:65, :], dn[64:65, :], reads=["dn"], writes=["dn"])
            P.mm(B[6][0:64, :], onesf[64:65, 0:64], dn[64:65, :], reads=["onesf", "dn"], writes=["B6"])
            P.cp(bc[:, :], B[6][0:64, :], reads=["B6"], writes=["bc"], eng="act")
            return bc

        def store(src_ap, key, row0):
            P.dma("pool", oT_d[row0:row0 + 64, t0:t0 + 512], src_ap, "st_" + key, reads=[key])

        sc_mla = 96.0 ** -0.5

        def mla_qk(i):
            P.mm(B[i % 4][:, :], KA[0:96, i * 128:(i + 1) * 128], QA[cb][0:96, :], reads=[f"KA{i // 4}", qak], writes=[f"B{i % 4}"])

        def mla_exp(i):
            p = i % 3
            P.act(Pt[p][:], B[i % 4][:, :], AF.Exp, scale=sc_mla, reads=[f"B{i % 4}"], writes=[f"Pt{p}"])
            if i >= 4 * c:
                P.tt(Pt[p][:], Pt[p][:], mI[i - 4 * c][:], ALU.mult, reads=[f"Pt{p}", f"mI{i - 4 * c}"], writes=[f"Pt{p}"])

        def mla_pv(i):
            p = i % 3
            P.mm(B[4][0:65, :], Va[:, i, 0:65], Pt[p][:], start=(i == 0), stop=(i == nkb - 1),
                 reads=[f"Va{i}", "Va1", f"Pt{p}"], writes=["B4"])

        pipeline(nkb, [(2, mla_qk), (0, mla_exp), (0, mla_pv)])
        finalize_norm(4, 0)
        o = ob[oi[0] % 3]; ok = f"ob{oi[0] % 3}"; oi[0] += 1
        P.tt(o[:, :], B[4][0:64, :], bc[:, :], ALU.mult, reads=["B4", "bc"], writes=[ok])
        store(o[:, :], ok, 0)

        sc_df = 32.0 ** -0.5
        P.ts(btab[:, 0:nkb], posKf[:, 0:nkb], pmidf[:, c:c + 1], cst[:, 2:3], ALU.subtract, op1=ALU.mult,
             reads=["posKf", "pmidf", "cst"], writes=["btab"])
        for mp in range(2):
            if mp == 0:
                Kt, Qt, kkey, qkey, p0, p1 = KB, QB[cb], "KB", f"QB{cb}", 64, 96
            else:
                Kt, Qt, kkey, qkey, p0, p1 = KC, QC[cb], "KC", f"QC{cb}", 0, 32
            obank = 4 + mp

            def df_qk(i, Kt=Kt, Qt=Qt, kkey=kkey, qkey=qkey, p0=p0, p1=p1):
                P.mm(B[i % 4][:, :], Kt[p0:p1, i * 128:(i + 1) * 128], Qt[p0:p1, :], reads=[f"{kkey}{i // 4}", qkey], writes=[f"B{i % 4}"])

            def df_exp(i):
                p = i % 3
                P.act(Pt[p][:], B[i % 4][:, :], AF.Exp, scale=sc_df, bias=btab[:, i:i + 1], reads=[f"B{i % 4}", "btab"], writes=[f"Pt{p}"])
                if i >= 4 * c:
                    P.tt(Pt[p][:], Pt[p][:], mI[i - 4 * c][:], ALU.mult, reads=[f"Pt{p}", f"mI{i - 4 * c}"], writes=[f"Pt{p}"])

            def df_pv(i, obank=obank):
                p = i % 3
                P.mm(B[obank][0:65, :], Vdf[:, i, 0:65], Pt[p][:], start=(i == 0), stop=(i == nkb - 1),
                     reads=[f"Vdf{i}", "Vdf1", f"Pt{p}"], writes=[f"B{obank}"])

            pipeline(nkb, [(2, df_qk), (0, df_exp), (0, df_pv)])
        finalize_norm(4, 0)
        P.tt(fa[:, :], B[4][0:64, :], bc[:, :], ALU.mult, reads=["B4", "bc"], writes=["fa"])
        finalize_norm(5, 0)
        P.tt(fb[:, :], B[5][0:64, :], bc[:, :], ALU.mult, reads=["B5", "bc"], writes=["fb"])
        P.stt(fa[:, :], fb[:, :], neglam, fa[:, :], ALU.mult, ALU.add, reads=["fa", "fb", "small"], writes=["fa"])
        P.act(fsq[:, :], fa[:, :], AF.Square, reads=["fa"], writes=["fsq"])
        P.mm(B[6][0:64, :], onesb[0:64, 0:64], fsq[:, :], reads=["onesb", "fsq"], writes=["B6"])
        P.act(fb[:, :], B[6][0:64, :], AF.Sqrt, scale=1.0 / 64, bias=cst[0:64, 12:13], reads=["B6", "cst", "fb"], writes=["fb"])
        P.recip(fb[:, :], fb[:, :], reads=["fb"], writes=["fb"])
        P.tt(fa[:, :], fa[:, :], fb[:, :], ALU.mult, reads=["fa", "fb"], writes=["fa"])
        o = ob[oi[0] % 3]; ok = f"ob{oi[0] % 3}"; oi[0] += 1
        P.ts(o[:, :], fa[:, :], cst[0:64, 13:14], cst[0:64, 8:9], ALU.mult, op1=ALU.mult, reads=["fa", "cst"], writes=[ok])
        store(o[:, :], ok, 256)

        order = list(range(nkb - 1, -1, -1))
        qbk = f"QB{cb}"

        def sb_qk(i):
            kb = order[i]
            P.mm(B[i % 2][:, :], KB[0:64, kb * 128:(kb + 1) * 128], QB[cb][0:64, :], reads=[f"KB{kb // 4}", qbk], writes=[f"B{i % 2}"])

        def sb_sp(i):
            kb = order[i]
            e = i % 2
            P.act(ef[0][:], B[i % 2][:, :], AF.Exp, scale=0.125, reads=[f"B{i % 2}"], writes=["ef0"])
            P.act(spb[e][:], ef[0][:], AF.Ln, bias=1.0, reads=["ef0"], writes=[f"spb{e}"])
            if kb >= 4 * c:
                P.tt(spb[e][:], spb[e][:], mS[kb - 4 * c][:], ALU.mult, reads=[f"spb{e}", f"mS{kb - 4 * c}"], writes=[f"spb{e}"])

        def sb_A(i):
            kb = order[i]
            e = i % 2
            bk = 2 + (i % 2)
            last_is_R = i > 0
            P.mm(B[bk][:, :], KB[0:64, kb * 128:(kb + 1) * 128], QB[cb][0:64, :], start=True, stop=False,
                 reads=[f"KB{kb // 4}", qbk], writes=[f"B{bk}"])
            P.mm(B[bk][:, :], Um[:, :], spb[e][:], start=False, stop=not last_is_R, reads=["Um", f"spb{e}"], writes=[f"B{bk}"])
            if last_is_R:
                P.mm(B[bk][:, :], On8[:, :], Rb[:], start=False, stop=True, reads=["On8", "Rb"], writes=[f"B{bk}"])
            if i == 0:
                P.cp(Rb[:], spb[e][:], reads=[f"spb{e}", "Rb"], writes=["Rb"], eng="pool")
            else:
                P.tt(Rb[:], Rb[:], spb[e][:], ALU.add, reads=[f"spb{e}", "Rb"], writes=["Rb"], eng="pool")

        def sb_exp2(i):
            kb = order[i]
            p = i % 3
            bk = 2 + (i % 2)
            P.act(Pt[p][:], B[bk][:, :], AF.Exp, scale=0.125, reads=[f"B{bk}"], writes=[f"Pt{p}"])
            if kb >= 4 * c:
                P.tt(Pt[p][:], Pt[p][:], mS[kb - 4 * c][:], ALU.mult, reads=[f"Pt{p}", f"mS{kb - 4 * c}"], writes=[f"Pt{p}"])

        def sb_pv(i):
            kb = order[i]
            p = i % 3
            P.mm(B[4][0:64, :], Vsb[:, kb, 0:64], Pt[p][:], start=(i == 0), stop=(i == nkb - 1),
                 reads=[f"Vsb{kb}", f"Pt{p}"], writes=["B4"])

        pipeline(nkb, [(2, sb_qk), (1, sb_sp), (1, sb_A), (0, sb_exp2), (0, sb_pv)])
        o = ob[oi[0] % 3]; ok = f"ob{oi[0] % 3}"; oi[0] += 1
        P.cp(o[:, :], B[4][0:64, :], reads=["B4"], writes=[ok], eng="act")
        store(o[:, :], ok, 64)

        sc_sw = 64.0 ** -0.5
        qsk = f"QSW{cb}"
        it = 0
        for jn in range(4):
            n = 4 * c + jn
            kbs = [kb for kb in (n - 1, n) if kb >= 0]
            for ki, kb in enumerate(kbs):
                which = 0 if kb == n - 1 else 1
                sbk = it % 2
                p = it % 3
                it += 1
                kslot = (kb // 4) % 2
                kof = (kb % 4) * 128
                for hh in range(2):
                    P.mm(B[sbk][:, hh * 128:(hh + 1) * 128], KSW[0:64, kslot, kof:kof + 128], QSW[cb][0:64, hh, jn * 128:(jn + 1) * 128],
                         reads=[f"KSW{kslot}", qsk], writes=[f"B{sbk}"])
                for hh in range(2):
                    P.act(Pt[p][:, hh * 128:(hh + 1) * 128], B[sbk][:, hh * 128:(hh + 1) * 128], AF.Exp, scale=sc_sw,
                          bias=bsw[hh][which][:, n:n + 1], reads=[f"B{sbk}", f"bsw{hh}{which}"], writes=[f"Pt{p}"])
                P.tt(Pt[p][:, 0:256], Pt[p][:, 0:256], mSW[which][:, :], ALU.mult, reads=[f"Pt{p}", f"mSW{which}"], writes=[f"Pt{p}"])
                for hh in range(2):
                    P.mm(B[4 + hh][0:65, jn * 128:(jn + 1) * 128], Vsw[:, kb % 8, 0:65], Pt[p][:, hh * 128:(hh + 1) * 128],
                         start=(ki == 0), stop=(ki == len(kbs) - 1), reads=[f"Vsw{kb % 8}", "Vsw1", f"Pt{p}"], writes=[f"B{4 + hh}"])
        for hh in range(2):
            finalize_norm(4 + hh, 0, extra_den=(sinkrow[hh][64:65, :], f"sinkrow{hh}"))
            o = ob[oi[0] % 3]; ok = f"ob{oi[0] % 3}"; oi[0] += 1
            P.tt(o[:, :], B[4 + hh][0:64, :], bc[:, :], ALU.mult, reads=[f"B{4 + hh}", "bc"], writes=[ok])
            store(o[:, :], ok, 128 + 64 * hh)

    P.emit()
    return nc

ALPHA = 8.0 ** 0.25
TOK = 2048
NG = TOK // 512
BR_CH = (2, 2, 4, 2)


def build_B1():
    nc = bass.Bass("TRN2", target_bir_lowering=False)
    P = Prog(nc)

    def din(name, shape, dt=F32):
        return nc.dram_tensor(name, list(shape), dt, kind="ExternalInput").ap()

    def dout(name, shape, dt=F32):
        return nc.dram_tensor(name, list(shape), dt, kind="ExternalOutput").ap()

    xT_d = din("xT", [1024, TOK])
    x_d = din("x", [TOK, 1024])
    oT_d = din("oT", [1280, TOK], BF16)
    wg_d = din("w_gate", [1024, 4096])
    bg_d = din("bgT", [128, 32])
    wbr_d = din("w_br", [1280, 1024])
    wo_d = din("w_out", [1024, 1024])
    lng_d = din("lng", [128, 1024])
    lnb_d = din("lnb", [128, 1024])
    wr_d = din("w_router", [1024, 32])
    rb_d = din("rbias", [128, 32])
    y1_d = dout("y1", [TOK, 1024])
    x2T_d = dout("x2T", [1024, TOK], BF16)
    gw_d = dout("gw", [TOK, 32])

    wg = P.sb([128, 8, 4096], BF16, "wg")
    wbr = P.sb([128, 10, 1024], BF16, "wbr")
    wo = P.sb([128, 8, 1024], BF16, "wo")
    stg = [P.sb([128, 1024], F32, f"stg{i}") for i in range(2)]
    bg = P.sb([128, 32], F32, "bg")
    lng = P.sb([128, 1024], F32, "lng")
    lnb = P.sb([128, 1024], F32, "lnb")
    wr = P.sb([128, 8, 32], F32, "wr")
    rb = P.sb([128, 32], F32, "rb")
    ioi = P.sb([128, 128], I32, "ioi")
    iof = P.sb([128, 128], F32, "iof")
    ident = P.sb([128, 128], F32, "ident")
    epsc = P.sb([128, 1], F32, "epsc")
    xs = [P.sb([128, 512], F32, f"xs{i}") for i in range(2)]
    xg = [P.sb([128, 8, 512], BF16, f"xg{i}") for i in range(2)]
    og = P.sb([128, 10, 512], BF16, "og")
    mT = P.sb([128, 8, 512], BF16, "mT")
    gs = [P.sb([128, 512], F32, f"gs{i}") for i in range(2)]
    macc = P.sb([128, 512], F32, "macc")
    mtmp = P.sb([128, 512], F32, "mtmp")
    xt = [P.sb([128, 1024], F32, f"xt{i}") for i in range(2)]
    yb = P.sb([128, 1024], F32, "yb")
    x2 = P.sb([128, 1024], F32, "x2")
    y1 = [P.sb([128, 1024], F32, f"y1_{i}") for i in range(2)]
    x2Tf = P.sb([128, 8, 128], F32, "x2Tf")
    x2Tb = [P.sb([128, 8, 128], BF16, f"x2Tb{i}") for i in range(2)]
    st = P.sb([128, 16], F32, "st")
    sc = P.sb([128, 32], F32, "sc")
    bi = P.sb([128, 32], F32, "bi")
    pr = P.sb([128, 6, 8], F32, "pr")
    pm = P.sb([128, 6, 8], F32, "pm")
    gsum = P.sb([128, 8], F32, "gsum")
    thr = P.sb([128, 8], F32, "thr")
    gsel = P.sb([128, 8], F32, "gsel")
    em = P.sb([128, 32], F32, "em")
    gwt = [P.sb([128, 32], F32, f"gwt{i}") for i in range(2)]
    B = [P.ps([128, 512], F32, f"bank{i}") for i in range(8)]
    P.excl = set(f"B{i}" for i in range(8))

    for i, (t, d_, k) in enumerate(((bg, bg_d, "bg"), (lng, lng_d, "lng"), (lnb, lnb_d, "lnb"), (rb, rb_d, "rb"))):
        P.dma("sp", t[:], d_[:, :], f"c{i}", writes=[k])
    P.dma("sp", wr[:], wr_d.rearrange("(dc p) e -> p dc e", p=128), "c9", writes=["wr"])
    P.op("pool", lambda e: e.iota(ioi[:], [[1, 128]], base=0, channel_multiplier=-1), writes=["ioi"])
    P.cp(iof[:], ioi[:], reads=["ioi"], writes=["iof"])
    P.ts(ident[:], iof[:], 0.0, None, ALU.is_equal, reads=["iof"], writes=["ident"])
    P.memset(epsc[:], LN_EPS, writes=["epsc"])
    si = [0]

    def load_cast(dst, src, cols, key):
        i = si[0] % 2
        si[0] += 1
        P.dma("sp", stg[i][:, 0:cols], src, f"stg{i}", writes=[f"stg{i}"])
        P.cp(dst, stg[i][:, 0:cols], reads=[f"stg{i}"], writes=[key], eng="pool" if si[0] % 2 else "dve")

    for dc in range(8):
        for q in range(4):
            load_cast(wg[:, dc, q * 1024:(q + 1) * 1024], wg_d[dc * 128:(dc + 1) * 128, q * 1024:(q + 1) * 1024], 1024, "wg")
    for ch in range(10):
        load_cast(wbr[:, ch, :], wbr_d[ch * 128:(ch + 1) * 128, :], 1024, "wbr")
    for dc in range(8):
        load_cast(wo[:, dc, :], wo_d[dc * 128:(dc + 1) * 128, :], 1024, "wo")

    br_off = [0, 2, 4, 8]
    xcnt = [0]
    bk = [0]
    tcount = [0]
    for g in range(NG):
        t0 = g * 512
        X = xg[g % 2]
        xk = f"xg{g % 2}"
        for dc in range(8):
            i = xcnt[0] % 2
            xcnt[0] += 1
            P.dma("sp", xs[i][:], xT_d[dc * 128:(dc + 1) * 128, t0:t0 + 512], f"xs{i}", writes=[f"xs{i}"])
            P.cp(X[:, dc, :], xs[i][:], reads=[f"xs{i}"], writes=[xk], eng="pool" if dc % 2 else "dve")
        P.dma("sp", og[:], oT_d[:, t0:t0 + 512].rearrange("(ch p) t -> p ch t", p=128), "og", writes=["og"])
        for fc in range(8):
            for i in range(4):
                gb = bk[0] % 2
                rbk = 2 + bk[0] % 2
                bk[0] += 1
                col = i * 1024 + fc * 128
                for dc in range(8):
                    P.mm(B[gb][:, :], wg[:, dc, col:col + 128], X[:, dc, :], start=(dc == 0), stop=(dc == 7),
                         reads=["wg", xk], writes=[f"B{gb}"])
                gi = bk[0] % 2
                P.act(gs[gi][:], B[gb][:, :], AF.Sigmoid, bias=bg[:, i * 8 + fc:i * 8 + fc + 1], reads=[f"B{gb}", "bg"], writes=[f"gs{gi}"])
                n = BR_CH[i]
                for k in range(n):
                    ch = br_off[i] + k
                    P.mm(B[rbk][:, :], wbr[:, ch, fc * 128:(fc + 1) * 128], og[:, ch, :], start=(k == 0), stop=(k == n - 1),
                         reads=["wbr", "og"], writes=[f"B{rbk}"])
                if i == 0:
                    P.tt(macc[:], gs[gi][:], B[rbk][:, :], ALU.mult, reads=[f"gs{gi}", f"B{rbk}"], writes=["macc"])
                else:
                    P.tt(mtmp[:], gs[gi][:], B[rbk][:, :], ALU.mult, reads=[f"gs{gi}", f"B{rbk}"], writes=["mtmp"])
                    if i < 3:
                        P.tt(macc[:], macc[:], mtmp[:], ALU.add, reads=["macc", "mtmp"], writes=["macc"], eng="pool")
                    else:
                        P.tt(mT[:, fc, :], macc[:], mtmp[:], ALU.add, reads=["macc", "mtmp"], writes=["mT"], eng="pool")
        for j in range(4):
            tt_ = tcount[0]
            tcount[0] += 1
            tok0 = t0 + j * 128
            xb = xt[tt_ % 2]
            xbk = f"xt{tt_ % 2}"
            P.dma("sp", xb[:], x_d[tok0:tok0 + 128, :], xbk, writes=[xbk])
            for ct in range(2):
                bnk = 4 + ct
                for fc in range(8):
                    P.mm(B[bnk][:, :], mT[:, fc, j * 128:(j + 1) * 128], wo[:, fc, ct * 512:(ct + 1) * 512], start=(fc == 0), stop=(fc == 7),
                         reads=["mT", "wo"], writes=[f"B{bnk}"])
                P.stt(yb[:, ct * 512:(ct + 1) * 512], xb[:, ct * 512:(ct + 1) * 512], ALPHA, B[bnk][:, :], ALU.mult, ALU.add,
                      reads=[xbk, f"B{bnk}"], writes=["yb"])
            P.op("dve", lambda e: e.bn_stats(st[:, 0:6], yb[:, 0:512]), reads=["yb"], writes=["st"])
            P.op("dve", lambda e: e.bn_stats(st[:, 6:12], yb[:, 512:1024]), reads=["yb"], writes=["st"])
            P.op("dve", lambda e: e.bn_aggr(st[:, 12:14], st[:, 0:12]), reads=["st"], writes=["st"])
            P.act(st[:, 14:15], st[:, 13:14], AF.Sqrt, bias=epsc[:, 0:1], reads=["st", "epsc"], writes=["st"])
            P.recip(st[:, 15:16], st[:, 14:15], reads=["st"], writes=["st"])
            P.ts(x2[:], yb[:], st[:, 12:13], st[:, 15:16], ALU.subtract, op1=ALU.mult, reads=["yb", "st"], writes=["x2"])
            P.tt(x2[:], x2[:], lng[:], ALU.mult, reads=["x2", "lng"], writes=["x2"], eng="pool")
            P.tt(x2[:], x2[:], lnb[:], ALU.add, reads=["x2", "lnb"], writes=["x2"])
            yo = y1[tt_ % 2]
            yk = f"y1_{tt_ % 2}"
            P.ts(yo[:], x2[:], ALPHA, None, ALU.mult, reads=["x2"], writes=[yk], eng="pool")
            P.dma("pool", y1_d[tok0:tok0 + 128, :], yo[:], "st_" + yk, reads=[yk])
            for dc in range(8):
                bnk = 6 + dc // 4
                P.tr(B[bnk][:, (dc % 4) * 128:(dc % 4 + 1) * 128], x2[:, dc * 128:(dc + 1) * 128], ident[:],
                     reads=["x2", "ident"], writes=[f"B{bnk}"])
            xtb = x2Tb[tt_ % 2]
            xtk = f"x2Tb{tt_ % 2}"
            for hb in range(2):
                P.cp(x2Tf[:, hb * 4:(hb + 1) * 4, :], B[6 + hb][:, :], reads=[f"B{6 + hb}"], writes=["x2Tf"], eng="act")
                P.cp(xtb[:, hb * 4:(hb + 1) * 4, :], B[6 + hb][:, :], reads=[f"B{6 + hb}"], writes=[xtk])
            P.dma("pool", x2T_d[:, tok0:tok0 + 128].rearrange("(dc p) t -> p dc t", p=128), xtb[:], "st_" + xtk, reads=[xtk])
            for dc in range(8):
                P.mm(B[6][:, 0:32], x2Tf[:, dc, :], wr[:, dc, :], start=(dc == 0), stop=(dc == 7), reads=["x2Tf", "wr"], writes=["B6"])
            P.act(sc[:], B[6][:, 0:32], AF.Sigmoid, reads=["B6"], writes=["sc"])
            P.tt(bi[:], sc[:], rb[:], ALU.add, reads=["sc", "rb"], writes=["bi"])
            v = bi[:, :].rearrange("p (g e) -> p g e", e=4)
            pairs = ((0, 1), (0, 2), (0, 3), (1, 2), (1, 3), (2, 3))
            for k, (a, b_) in enumerate(pairs):
                P.tt(pr[:, k, :], v[:, :, a], v[:, :, b_], ALU.add, reads=["bi"], writes=["pr"])
                P.tt(pm[:, k, :], v[:, :, a], v[:, :, b_], ALU.min, reads=["bi"], writes=["pm"])
            P.tt(gsum[:], pr[:, 0, :], pr[:, 1, :], ALU.max, reads=["pr"], writes=["gsum"])
            P.tt(thr[:], pm[:, 0, :], pm[:, 1, :], ALU.max, reads=["pm"], writes=["thr"])
            for k in range(2, 6):
                P.tt(gsum[:], gsum[:], pr[:, k, :], ALU.max, reads=["pr", "gsum"], writes=["gsum"])
                P.tt(thr[:], thr[:], pm[:, k, :], ALU.max, reads=["pm", "thr"], writes=["thr"])
            P.op("dve", lambda e: e.tensor_reduce(st[:, 0:1], gsum[:], AX.X, ALU.max), reads=["gsum", "st"], writes=["st"])
            P.ts(gsel[:], gsum[:], st[:, 0:1], None, ALU.is_ge, reads=["gsum", "st"], writes=["gsel"])
            emv = em[:, :].rearrange("p (g e) -> p g e", e=4)
            for a in range(4):
                P.tt(emv[:, :, a], v[:, :, a], thr[:], ALU.is_ge, reads=["bi", "thr"], writes=["em"])
                P.tt(emv[:, :, a], emv[:, :, a], gsel[:], ALU.mult, reads=["em", "gsel"], writes=["em"])
            P.tt(em[:], em[:], sc[:], ALU.mult, reads=["em", "sc"], writes=["em"])
            P.op("dve", lambda e: e.tensor_reduce(st[:, 1:2], em[:], AX.X, ALU.add), reads=["em", "st"], writes=["st"])
            P.recip(st[:, 2:3], st[:, 1:2], reads=["st"], writes=["st"])
            go = gwt[tt_ % 2]
            gk = f"gwt{tt_ % 2}"
            P.ts(go[:], em[:], st[:, 2:3], None, ALU.mult, reads=["em", "st"], writes=[gk])
            P.dma("pool", gw_d[tok0:tok0 + 128, :], go[:], "st_" + gk, reads=[gk])
    P.emit()
    return nc


def build_B2():
    nc = bass.Bass("TRN2", target_bir_lowering=False)
    P = Prog(nc)

    def din(name, shape, dt=F32):
        return nc.dram_tensor(name, list(shape), dt, kind="ExternalInput").ap()

    y1_d = din("y1", [TOK, 1024])
    x2T_d = din("x2T", [1024, TOK], BF16)
    gw_d = din("gw", [TOK, 32])
    w1_d = din("w1", [32, 1024, 256])
    w3_d = din("w3", [32, 1024, 256])
    w2_d = din("w2", [32, 256, 1024])
    lng_d = din("lng", [128, 1024])
    lnb_d = din("lnb", [128, 1024])
    out_d = nc.dram_tensor("xo", [TOK, 1024], F32, kind="ExternalOutput").ap()

    NTL = TOK // 128
    acc = P.sb([128, NTL, 1024], F32, "acc")
    x2T = P.sb([128, 8, TOK], BF16, "x2T")
    gw = P.sb([128, NTL, 32], F32, "gw")
    lng = P.sb([128, 1024], F32, "lng")
    lnb = P.sb([128, 1024], F32, "lnb")
    epsc = P.sb([128, 1], F32, "epsc")
    stg = [P.sb([128, 2048], F32, f"stg{i}") for i in range(3)]
    w13 = [P.sb([128, 8, 512], BF16, f"w13_{i}") for i in range(2)]
    w2 = [P.sb([128, 2, 1024], BF16, f"w2_{i}") for i in range(2)]
    sl = [P.sb([128, 2, 512], F32, f"sl{i}") for i in range(2)]
    hT = [P.sb([128, 2, 512], BF16, f"hT{i}") for i in range(2)]
    st = P.sb([128, 16], F32, "st")
    xo = [P.sb([128, 1024], F32, f"xo{i}") for i in range(2)]
    B = [P.ps([128, 512], F32, f"bank{i}") for i in range(8)]
    P.excl = set(f"B{i}" for i in range(8))

    P.dma("sp", lng[:], lng_d[:, :], "c0", writes=["lng"])
    P.dma("sp", lnb[:], lnb_d[:, :], "c1", writes=["lnb"])
    P.memset(epsc[:], LN_EPS, writes=["epsc"])
    P.dma("sp", x2T[:], x2T_d.rearrange("(dc p) t -> p dc t", p=128), "c2", writes=["x2T"])
    P.dma("sp", gw[:], gw_d.rearrange("(j p) e -> p j e", p=128), "c3", writes=["gw"])
    for j in range(NTL):
        P.dma("sp", acc[:, j, :], y1_d[j * 128:(j + 1) * 128, :], "c4", writes=[f"acc{j}"])

    si = [0]
    ybk = [0]
    for e in range(32):
        eb = e % 2
        wk = f"w13_{eb}"
        w2k = f"w2_{eb}"
        for which, src in enumerate((w1_d, w3_d)):
            i = si[0] % 3
            si[0] += 1
            P.dma("sp", stg[i][:, :].rearrange("p (dc f) -> p dc f", dc=8), src[e].rearrange("(dc p) f -> p dc f", p=128), f"stg{i}", writes=[f"stg{i}"])
            P.cp(w13[eb][:, :, which * 256:(which + 1) * 256], stg[i][:, :].rearrange("p (dc f) -> p dc f", dc=8), reads=[f"stg{i}"], writes=[wk],
                 eng="pool")
        i = si[0] % 3
        si[0] += 1
        P.dma("sp", stg[i][:, :].rearrange("p (fc d) -> p fc d", fc=2), w2_d[e].rearrange("(fc p) d -> p fc d", p=128), f"stg{i}", writes=[f"stg{i}"])
        P.cp(w2[eb][:, :, :], stg[i][:, :].rearrange("p (fc d) -> p fc d", fc=2), reads=[f"stg{i}"], writes=[w2k], eng="pool")
        for g in range(NG):
            t0 = g * 512
            hb = g % 2
            for q in range(4):
                for dc in range(8):
                    P.mm(B[q][:, :], w13[eb][:, dc, q * 128:(q + 1) * 128], x2T[:, dc, t0:t0 + 512], start=(dc == 0), stop=(dc == 7),
                         reads=[wk, "x2T"], writes=[f"B{q}"])
            for fc in range(2):
                P.act(sl[hb][:, fc, :], B[fc][:, :], AF.Silu, reads=[f"B{fc}"], writes=[f"sl{hb}"])
                P.tt(hT[hb][:, fc, :], sl[hb][:, fc, :], B[2 + fc][:, :], ALU.mult, reads=[f"sl{hb}", f"B{2 + fc}"], writes=[f"hT{hb}"])
            for j in range(4):
                tj = g * 4 + j
                for ct in range(2):
                    bnk = 4 + ybk[0] % 4
                    ybk[0] += 1
                    for fc in range(2):
                        P.mm(B[bnk][:, :], hT[hb][:, fc, j * 128:(j + 1) * 128], w2[eb][:, fc, ct * 512:(ct + 1) * 512], start=(fc == 0), stop=(fc == 1),
                             reads=[f"hT{hb}", w2k], writes=[f"B{bnk}"])
                    P.stt(acc[:, tj, ct * 512:(ct + 1) * 512], B[bnk][:, :], gw[:, tj, e:e + 1], acc[:, tj, ct * 512:(ct + 1) * 512], ALU.mult, ALU.add,
                          reads=[f"B{bnk}", "gw", f"acc{tj}"], writes=[f"acc{tj}"])
    for j in range(NTL):
        a = acc[:, j, :]
        ak = f"acc{j}"
        P.op("dve", lambda e, a=a: e.bn_stats(st[:, 0:6], a[:, 0:512]), reads=[ak, "st"], writes=["st"])
        P.op("dve", lambda e, a=a: e.bn_stats(st[:, 6:12], a[:, 512:1024]), reads=[ak], writes=["st"])
        P.op("dve", lambda e: e.bn_aggr(st[:, 12:14], st[:, 0:12]), reads=["st"], writes=["st"])
        P.act(st[:, 14:15], st[:, 13:14], AF.Sqrt, bias=epsc[:, 0:1], reads=["st", "epsc"], writes=["st"])
        P.recip(st[:, 15:16], st[:, 14:15], reads=["st"], writes=["st"])
        o = xo[j % 2]
        ok = f"xo{j % 2}"
        P.ts(o[:], a, st[:, 12:13], st[:, 15:16], ALU.subtract, op1=ALU.mult, reads=[ak, "st"], writes=[ok])
        P.tt(o[:], o[:], lng[:], ALU.mult, reads=[ok, "lng"], writes=[ok], eng="pool")
        P.tt(o[:], o[:], lnb[:], ALU.add, reads=[ok, "lnb"], writes=[ok])
        P.dma("pool", out_d[j * 128:(j + 1) * 128, :], o[:], "st_" + ok, reads=[ok])
    P.emit()
    return nc


def alibi(n):
    return (2.0 ** (-8.0 * np.arange(1, n + 1, dtype=np.float32) / n)).astype(np.float32)


def prepA_shared(inp, S):
    pos = np.ascontiguousarray(inp["positions"][:S]).astype(np.int32)
    NB = S // 128
    NCH = S // 512
    d = {}
    d["posK"] = np.ascontiguousarray(pos.reshape(NB, 128).T)
    d["pmidB"] = np.ascontiguousarray(np.broadcast_to(pos[256::512][None, :], (128, NCH)))
    d["refB"] = np.ascontiguousarray(np.broadcast_to(pos[0::128][None, :], (128, NB)))
    d["pos32"] = np.ascontiguousarray(np.broadcast_to(pos[None, :], (32, S)))
    d["posrow"] = np.ascontiguousarray(pos[None, :])
    d["refrow"] = np.ascontiguousarray(np.repeat(pos[0::128], 128)[None, :])
    return d


def prepA_layer_head(inp, l, h):
    w_in = inp["w_in"][l]
    cols = np.zeros(NT, dtype=np.int64)

    def put(name, idx):
        off, m = G[name]
        assert len(idx) == m
        cols[off:off + m] = idx

    put("cq0", np.arange(0, 128))
    put("cq1", np.arange(128, 256))
    put("ckv", np.arange(256, 384))
    kr = np.arange(384, 416)
    krs = np.concatenate([kr[16:], kr[:16]])
    put("kr", np.concatenate([np.arange(0, 64), kr]))
    put("krs", np.concatenate([np.arange(0, 64), krs]))
    sb0 = 416
    sbq = sb0 + h * 64 + np.arange(64)
    sbk = sb0 + 256 + h * 64 + np.arange(64)
    sbv = sb0 + 512 + h * 64 + np.arange(64)
    swq0 = 1184
    swqA = swq0 + (2 * h) * 64 + np.arange(64)
    swqB = swq0 + (2 * h + 1) * 64 + np.arange(64)
    swk = 1696 + (h // 2) * 64 + np.arange(64)
    swv = 1696 + 128 + (h // 2) * 64 + np.arange(64)
    dfq1 = 1952 + h * 64 + np.arange(32)
    dfq2 = 1952 + h * 64 + 32 + np.arange(32)
    dfk1 = 2208 + h * 64 + np.arange(32)
    dfk2 = 2208 + h * 64 + 32 + np.arange(32)
    dfv = 2464 + h * 64 + np.arange(64)
    put("qB", np.concatenate([sbq, dfq1]))
    put("kB", np.concatenate([sbk, dfk1]))
    put("qC", dfq2)
    put("kC", dfk2)
    put("swqA", swqA)
    put("swqB", swqB)
    put("swk", swk)
    d = {}
    d["wT"] = np.ascontiguousarray(w_in[:, cols])
    d["wV"] = np.ascontiguousarray(w_in[:, np.concatenate([sbv, swv, dfv])])
    wq = inp["w_uq"][l][:, h * 96:(h + 1) * 96]
    perm = np.concatenate([np.arange(64), 64 + 16 + np.arange(16), 64 + np.arange(16)])
    d["wuq"] = np.ascontiguousarray(np.concatenate([wq, wq[:, perm]], axis=1))
    d["wukv"] = np.ascontiguousarray(inp["w_ukv"][l][:, h * 128:(h + 1) * 128])
    d["qn"] = np.ascontiguousarray(inp["mla_q_norm"][l].reshape(2, 128).T)
    d["kvn"] = np.ascontiguousarray(inp["mla_kv_norm"][l].reshape(128, 1))
    cst = np.zeros((128, 16), np.float32)
    half = 16
    inv_freq = (np.float32(10000.0) ** (-np.arange(half, dtype=np.float32) / np.float32(half))).astype(np.float32)
    cst[64:96, 0] = np.concatenate([inv_freq, inv_freq])
    cst[64:96, 1] = np.concatenate([-np.ones(16), np.ones(16)])
    cst[:, 2] = alibi(4)[h]
    cst[:, 3] = alibi(8)[2 * h]
    cst[:, 4] = alibi(8)[2 * h + 1]
    lam_init = 0.8 - 0.6 * math.exp(-0.3 * l)
    cst[:, 7] = lam_init
    cst[:, 8] = 1.0 - lam_init
    cst[:, 9] = inp["sw_sinks"][l][2 * h]
    cst[:, 10] = inp["sw_sinks"][l][2 * h + 1]
    cst[:, 11] = math.pi / 2
    cst[:, 12] = RMS_EPS
    cst[0:64, 13] = inp["df_subln"][l]
    d["cst"] = cst
    lqk = np.concatenate([inp["df_lq1"][l], inp["df_lk1"][l], inp["df_lq2"][l], inp["df_lk2"][l]])
    d["lqk"] = np.ascontiguousarray(np.broadcast_to(lqk[None, :], (64, 128))).astype(np.float32)
    return d

def prepB1_weights(inp, l):
    d = {}
    d["w_gate"] = np.ascontiguousarray(inp["w_gate"][l])
    d["bgT"] = np.ascontiguousarray(inp["b_gate"][l].reshape(4, 8, 128).transpose(2, 0, 1).reshape(128, 32))
    d["w_br"] = np.ascontiguousarray(np.concatenate([inp["w_br_a"][l], inp["w_br_b"][l], inp["w_br_c"][l], inp["w_br_d"][l]], axis=0))
    d["w_out"] = np.ascontiguousarray(inp["w_out"][l])
    d["lng"] = np.ascontiguousarray(np.broadcast_to(inp["ln1_g"][l][None, :], (128, 1024)))
    d["lnb"] = np.ascontiguousarray(np.broadcast_to(inp["ln1_b"][l][None, :], (128, 1024)))
    d["w_router"] = np.ascontiguousarray(inp["w_router"])
    d["rbias"] = np.ascontiguousarray(np.broadcast_to(inp["router_bias"][None, :], (128, 32)))
    return d

def prepB2_weights(inp, l):
    d = {}
    d["w1"] = np.ascontiguousarray(inp["moe_w1"][l])
    d["w3"] = np.ascontiguousarray(inp["moe_w3"][l])
    d["w2"] = np.ascontiguousarray(inp["moe_w2"][l])
    d["lng"] = np.ascontiguousarray(np.broadcast_to(inp["ln2_g"][l][None, :], (128, 1024)))
    d["lnb"] = np.ascontiguousarray(np.broadcast_to(inp["ln2_b"][l][None, :], (128, 1024)))
    return d

def assemble_oT(resA, nb=2, S=8192):
    out = np.zeros((1280, nb * S), dtype=resA[0]["oT"].dtype)
    for b in range(nb):
        for h in range(4):
            o = resA[b * 4 + h]["oT"]
            cs = slice(b * S, (b + 1) * S)
            out[h * 64:(h + 1) * 64, cs] = o[0:64]
            out[256 + h * 64:256 + (h + 1) * 64, cs] = o[64:128]
            out[512 + 2 * h * 64:512 + (2 * h + 1) * 64, cs] = o[128:192]
            out[512 + (2 * h + 1) * 64:512 + (2 * h + 2) * 64, cs] = o[192:256]
            out[1024 + h * 64:1024 + (h + 1) * 64, cs] = o[256:320]
    return out


_CACHE = {}


def _progs():
    if "A" not in _CACHE:
        _CACHE["A"] = build_phaseA(8192)
        _CACHE["B1"] = build_B1()
        _CACHE["B2"] = build_B2()
    return _CACHE["A"], _CACHE["B1"], _CACHE["B2"]


def kernel(**inp):
    inp = {k: np.asarray(v) for k, v in inp.items()}
    S = 8192
    ncA, ncB1, ncB2 = _progs()
    shared = prepA_shared(inp, S)
    xcur = np.ascontiguousarray(inp["x"].astype(np.float32).reshape(2 * S, 1024))
    cores = list(range(8))
    for l in range(4):
        maps = []
        for b in range(2):
            xT = np.ascontiguousarray(xcur[b * S:(b + 1) * S].T)
            for h in range(4):
                m = dict(shared)
                m.update(prepA_layer_head(inp, l, h))
                m["xT"] = xT
                maps.append(m)
        resA = run_bass_kernel_spmd(ncA, maps, core_ids=cores).results
        oT = assemble_oT([{"oT": np.asarray(r["oT"])} for r in resA])
        w = prepB1_weights(inp, l)
        maps = []
        for c in range(8):
            sl = slice(c * TOK, (c + 1) * TOK)
            m = dict(w)
            m["xT"] = np.ascontiguousarray(xcur[sl].T)
            m["x"] = np.ascontiguousarray(xcur[sl])
            m["oT"] = np.ascontiguousarray(oT[:, sl])
            maps.append(m)
        res1 = run_bass_kernel_spmd(ncB1, maps, core_ids=cores).results
        w = prepB2_weights(inp, l)
        maps = []
        for c in range(8):
            m = dict(w)
            for k in ("y1", "x2T", "gw"):
                m[k] = np.asarray(res1[c][k])
            maps.append(m)
        res2 = run_bass_kernel_spmd(ncB2, maps, core_ids=cores).results
        xcur = np.ascontiguousarray(np.concatenate([np.asarray(res2[c]["xo"]) for c in range(8)], axis=0))
    return xcur.reshape(2, S, 1024).astype(np.float32)
```

```python
import math
from concourse.bass_utils import run_bass_kernel_spmd
import numpy as np
from contextlib import ExitStack
import concourse.bass as bass
import concourse.mybir as mybir

F32 = mybir.dt.float32
BF16 = mybir.dt.bfloat16
I32 = mybir.dt.int32
AF = mybir.ActivationFunctionType
ALU = mybir.AluOpType
AX = mybir.AxisListType


class Prog:
    ENGS = ("pe", "act", "dve", "pool", "sp")

    def __init__(self, nc):
        self.nc = nc
        self.ops = {e: [] for e in self.ENGS}
        self.cnt = {e: 0 for e in self.ENGS}
        self.dcnt = {}
        self.waited = {}
        self.lastw = {}
        self.readers = {}
        self.stack = ExitStack()
        self.nsb = 0
        self.excl = set()

    def sb(self, shape, dt, name=None):
        self.nsb += 1
        return self.stack.enter_context(self.nc.sbuf_tensor((name + "_sbuf") if name else f"sb{self.nsb}", list(shape), dt))

    def ps(self, shape, dt=F32, name=None):
        self.nsb += 1
        return self.stack.enter_context(self.nc.psum_tensor(name or f"ps{self.nsb}", list(shape), dt))

    def _deps(self, eng, reads, writes):
        deps = set()
        for r in reads:
            if r in self.lastw:
                deps.add(self.lastw[r])
            if r in self.excl:
                for t in self.readers.get(r, ()):
                    if not (t[0] == "eng" and t[1] == eng):
                        deps.add(t)
        for w in writes:
            if w in self.lastw:
                deps.add(self.lastw[w])
            for t in self.readers.get(w, ()):
                deps.add(t)
        best = {}
        for (kind, key, val) in deps:
            if kind == "eng" and key == eng and eng in ("pe", "sp"):
                continue
            k = (kind, key)
            if val > best.get(k, 0):
                best[k] = val
        waits = []
        for k, val in best.items():
            if val > self.waited.get((eng, k), 0):
                self.waited[(eng, k)] = val
                waits.append((k, val))
        return waits

    def _mark(self, tok, reads, writes):
        for w in writes:
            self.lastw[w] = tok
            self.readers[w] = []
        for r in reads:
            self.readers.setdefault(r, []).append(tok)

    def op(self, eng, fn, reads=(), writes=()):
        waits = self._deps(eng, reads, writes)
        self.cnt[eng] += 1
        tok = ("eng", eng, self.cnt[eng])
        self.ops[eng].append((waits, fn, ("eng", eng)))
        self._mark(tok, reads, writes)
        return tok

    def dma(self, q, out, in_, sem, reads=(), writes=()):
        waits = self._deps(q, reads, writes)
        self.dcnt[sem] = self.dcnt.get(sem, 0) + 16
        tok = ("dma", sem, self.dcnt[sem])
        self.ops[q].append((waits, lambda e: e.dma_start(out=out, in_=in_), ("dma", sem)))
        self._mark(tok, reads, writes)
        return tok

    def mm(self, out, lhsT, rhs, start=True, stop=True, reads=(), writes=()):
        return self.op("pe", lambda e: e.matmul(out, lhsT, rhs, start=start, stop=stop), reads, writes)

    def tr(self, out, in_, ident, reads=(), writes=()):
        return self.op("pe", lambda e: e.transpose(out, in_, ident), reads, writes)

    def act(self, out, in_, func, bias=None, scale=None, accum_out=None, reads=(), writes=(), eng="act"):
        kw = {}
        if bias is not None:
            kw["bias"] = bias
        if scale is not None:
            kw["scale"] = scale
        if accum_out is not None:
            kw["accum_out"] = accum_out
        return self.op(eng, lambda e: e.activation(out, in_, func, **kw), reads, writes)

    def ts(self, out, in0, s1, s2, op0, op1=None, reads=(), writes=(), eng="dve", accum_out=None):
        kw = {}
        if op1 is not None:
            kw["op1"] = op1
        if accum_out is not None:
            kw["accum_out"] = accum_out
        return self.op(eng, lambda e: e.tensor_scalar(out, in0, s1, s2, op0, **kw), reads, writes)

    def tt(self, out, in0, in1, op, reads=(), writes=(), eng="dve"):
        return self.op(eng, lambda e: e.tensor_tensor(out, in0, in1, op), reads, writes)

    def stt(self, out, in0, scalar, in1, op0, op1, reads=(), writes=(), eng="dve"):
        return self.op(eng, lambda e: e.scalar_tensor_tensor(out, in0, scalar, in1, op0, op1), reads, writes)

    def cp(self, out, in_, reads=(), writes=(), eng="dve"):
        if eng == "act":
            return self.op(eng, lambda e: e.activation(out, in_, AF.Copy), reads, writes)
        return self.op(eng, lambda e: e.tensor_copy(out, in_), reads, writes)

    def recip(self, out, in_, reads=(), writes=(), eng="dve"):
        return self.op(eng, lambda e: e.reciprocal(out, in_), reads, writes)

    def memset(self, out, val, writes=(), eng="pool"):
        return self.op(eng, lambda e: e.memset(out, val), (), writes)

    def emit(self):
        nc = self.nc
        sems = {}
        for e in self.ENGS:
            sems[("eng", e)] = self.stack.enter_context(nc.semaphore("s_" + e))
        for s in self.dcnt:
            sems[("dma", s)] = self.stack.enter_context(nc.semaphore("d_" + s))
        final = [(("eng", e), self.cnt[e]) for e in self.ENGS if self.cnt[e] > 0 and e != "sp"]
        final += [(("dma", s), v) for s, v in self.dcnt.items()]
        ops = self.ops

        def run(e, name):
            for waits, fn, semk in ops[name]:
                for k, val in waits:
                    e.wait_ge(sems[k], val)
                ins = fn(e)
                if semk[0] == "dma":
                    ins.then_inc(sems[semk], 16)
                else:
                    ins.then_inc(sems[semk], 1)

        with nc.Block() as block:
            @block.tensor
            def _(e):
                run(e, "pe")

            @block.scalar
            def _(e):
                run(e, "act")

            @block.vector
            def _(e):
                run(e, "dve")

            @block.gpsimd
            def _(e):
                run(e, "pool")

            @block.sync
            def _(e):
                run(e, "sp")
                for k, val in final:
                    e.wait_ge(sems[k], val)
        self.stack.close()
        return nc

LN_EPS = 1e-5
RMS_EPS = 1e-6
MAGIC = 12582912.0
C1 = 6.28125
C2 = 2.0 * math.pi - 6.28125
G = {}
_off = 0
for _n, _m in (("cq0", 128), ("cq1", 128), ("ckv", 128), ("kr", 96), ("krs", 96), ("qB", 96), ("kB", 96),
               ("qC", 32), ("kC", 32), ("swqA", 64), ("swqB", 64), ("swk", 64)):
    G[_n] = (_off, _m)
    _off += _m
NT = _off


def pipeline(n, stages):
    mx = max(o for o, _ in stages)
    for t in range(-mx, n):
        for off, fn in stages:
            i = t + off
            if 0 <= i < n:
                fn(i)


def build_phaseA(S):
    nc = bass.Bass("TRN2", target_bir_lowering=False)
    P = Prog(nc)
    NCH = S // 512
    NB = S // 128

    def din(name, shape, dt=F32):
        return nc.dram_tensor(name, list(shape), dt, kind="ExternalInput").ap()

    xT_d = din("xT", [1024, S])
    wT_d = din("wT", [1024, NT])
    wV_d = din("wV", [1024, 192])
    wuq_d = din("wuq", [256, 192])
    wukv_d = din("wukv", [128, 128])
    qn_d = din("qn", [128, 2])
    kvn_d = din("kvn", [128, 1])
    cst_d = din("cst", [128, 16])
    lqk_d = din("lqk", [64, 128])
    posK_d = din("posK", [128, NB], I32)
    pmid_d = din("pmidB", [128, NCH], I32)
    ref_d = din("refB", [128, NB], I32)
    pos32_d = din("pos32", [32, S], I32)
    posrow_d = din("posrow", [1, S], I32)
    refrow_d = din("refrow", [1, S], I32)
    oT_d = nc.dram_tensor("oT", [320, S], BF16, kind="ExternalOutput").ap()

    wT = P.sb([128, 8, NT], BF16, "wT_sb")
    wV = P.sb([128, 8, 192], BF16, "wV_sb")
    wuq = P.sb([128, 2, 192], BF16, "wuq_sb")
    wukv = P.sb([128, 128], BF16, "wukv_sb")
    stg = [P.sb([128, 1024], F32, f"stg{i}") for i in range(2)]
    qn = P.sb([128, 2], F32, "qn_sb")
    kvn = P.sb([128, 1], F32, "kvn_sb")
    cst = P.sb([128, 16], F32, "cst_sb")
    lqk = P.sb([64, 128], F32, "lqk_sb")
    small = P.sb([128, 16], F32, "small")
    posKi = P.sb([128, NB], I32, "posKi")
    posKf = P.sb([128, NB], F32, "posKf")
    pmidi = P.sb([128, NCH], I32, "pmidi")
    pmidf = P.sb([128, NCH], F32, "pmidf")
    refi = P.sb([128, NB], I32, "refi")
    reff = P.sb([128, NB], F32, "reff")
    bsw = [[P.sb([128, NB], F32, f"bsw{h}{w}") for w in range(2)] for h in range(2)]
    btab = P.sb([128, NB], F32, "btab")
    onesb = P.sb([128, 128], BF16, "onesb")
    onesf = P.sb([128, 64], F32, "onesf")
    Um = P.sb([128, 128], BF16, "Um")
    On8 = P.sb([128, 128], BF16, "On8")
    ioi = P.sb([128, 512], I32, "ioi")
    iof = P.sb([128, 512], F32, "iof")
    mI = [P.sb([128, 512], BF16, f"mI{o}") for o in range(4)]
    mS = [P.sb([128, 512], BF16, f"mS{o}") for o in range(4)]
    mSW = [P.sb([128, 256], BF16, f"mSW{w}") for w in range(2)]
    KA = P.sb([128, S], BF16, "KA")
    KB = P.sb([128, S], BF16, "KB")
    KC = P.sb([32, S], BF16, "KC")
    KSW = P.sb([64, 2, 512], BF16, "KSW")
    Va = P.sb([128, NB, 65], BF16, "Va")
    Vsb = P.sb([128, NB, 65], BF16, "Vsb")
    Vdf = P.sb([128, NB, 65], BF16, "Vdf")
    Vsw = P.sb([128, 8, 65], BF16, "Vsw")
    xs = [P.sb([128, 512], F32, f"xs{i}") for i in range(2)]
    xbf = [P.sb([128, 8, 512], BF16, f"xbf{i}") for i in range(2)]
    QA = [P.sb([96, 512], BF16, f"QA{i}") for i in range(2)]
    QB = [P.sb([96, 512], BF16, f"QB{i}") for i in range(2)]
    QC = [P.sb([32, 512], BF16, f"QC{i}") for i in range(2)]
    QSW = [P.sb([64, 2, 512], BF16, f"QSW{i}") for i in range(2)]
    cqb = P.sb([128, 2, 512], BF16, "cqb")
    sqq = P.sb([128, 2, 512], BF16, "sqq")
    ckvb = P.sb([128, 512], BF16, "ckvb")
    sqkv = P.sb([128, 512], BF16, "sqkv")
    rq = P.sb([128, 512], F32, "rq")
    rkv = P.sb([128, 512], F32, "rkv")
    rp_i = P.sb([96, 512], I32, "rp_i")
    rp_a = P.sb([96, 512], F32, "rp_a")
    rp_b = P.sb([96, 512], F32, "rp_b")
    cosT = P.sb([96, 512], F32, "cosT")
    sinT = P.sb([96, 512], F32, "sinT")
    t1 = P.sb([96, 512], F32, "t1")
    t2 = P.sb([96, 512], F32, "t2")
    rowi = P.sb([65, 512], I32, "rowi")
    rowi2 = P.sb([65, 512], I32, "rowi2")
    rowf = P.sb([65, 512], F32, "rowf")
    sinkrow = [P.sb([65, 512], F32, f"sinkrow{h}") for h in range(2)]
    Pt = [P.sb([128, 512], BF16, f"Pt{i}") for i in range(3)]
    ef = [P.sb([128, 512], F32, "ef0")]
    spb = [P.sb([128, 512], BF16, f"spb{i}") for i in range(2)]
    Rb = P.sb([128, 512], BF16, "Rb")
    dn = P.sb([65, 512], F32, "dn")
    bc = P.sb([64, 512], F32, "bc")
    fa = P.sb([64, 512], F32, "fa")
    fb = P.sb([64, 512], F32, "fb")
    fsq = P.sb([64, 512], BF16, "fsq")
    ob = [P.sb([64, 512], BF16, f"ob{i}") for i in range(3)]
    rstok = P.sb([128, 4], F32, "rstok")
    B = [P.ps([128, 512], F32, f"bank{i}") for i in range(8)]
    P.excl = set(f"B{i}" for i in range(8))

    P.dma("sp", cst[:], cst_d[:, :], "c0", writes=["cst"])
    P.dma("sp", qn[:], qn_d[:, :], "c1", writes=["qn"])
    P.dma("sp", kvn[:], kvn_d[:, :], "c2", writes=["kvn"])
    P.dma("sp", lqk[:], lqk_d[:, :], "c3", writes=["lqk"])
    P.dma("sp", posKi[:], posK_d[:, :], "c4", writes=["posKi"])
    P.dma("sp", pmidi[:], pmid_d[:, :], "c5", writes=["pmidi"])
    P.dma("sp", refi[:], ref_d[:, :], "c6", writes=["refi"])
    P.cp(posKf[:], posKi[:], reads=["posKi"], writes=["posKf"])
    P.cp(pmidf[:], pmidi[:], reads=["pmidi"], writes=["pmidf"])
    P.cp(reff[:], refi[:], reads=["refi"], writes=["reff"])
    P.memset(onesb[:], 1.0, writes=["onesb"])
    P.memset(onesf[:], 1.0, writes=["onesf"])
    P.memset(On8[:], -8.0, writes=["On8"])
    P.memset(Va[:, :, 64:65], 1.0, writes=["Va1"])
    P.memset(Vsb[:, :, 64:65], 1.0, writes=["Vsb1"])
    P.memset(Vdf[:, :, 64:65], 1.0, writes=["Vdf1"])
    P.memset(Vsw[:, :, 64:65], 1.0, writes=["Vsw1"])
    P.op("pool", lambda e: e.iota(ioi[:], [[1, 512]], base=0, channel_multiplier=-1), writes=["ioi"])
    P.cp(iof[:], ioi[:], reads=["ioi"], writes=["iof"])
    for o in range(4):
        P.ts(mI[o][:], iof[:], float(128 * o), None, ALU.is_ge, reads=["iof"], writes=[f"mI{o}"])
        P.ts(mS[o][:], iof[:], float(128 * o + 1), None, ALU.is_ge, reads=["iof"], writes=[f"mS{o}"])
    for hh in range(2):
        P.ts(mSW[0][:, hh * 128:(hh + 1) * 128], iof[:, 0:128], -1.0, None, ALU.is_le, reads=["iof"], writes=["mSW0"])
        P.ts(mSW[1][:, hh * 128:(hh + 1) * 128], iof[:, 0:128], 0.0, None, ALU.is_ge, reads=["iof"], writes=["mSW1"])
    P.ts(Um[:], iof[:, 0:128], 0.0, -8.0, ALU.is_le, op1=ALU.mult, reads=["iof"], writes=["Um"])
    for hh in range(2):
        sl = cst[:, 3 + hh:4 + hh]
        if NB > 1:
            P.tt(bsw[hh][0][:, 1:NB], posKf[:, 0:NB - 1], reff[:, 1:NB], ALU.subtract, reads=["posKf", "reff"], writes=[f"bsw{hh}0"])
            P.ts(bsw[hh][0][:, 1:NB], bsw[hh][0][:, 1:NB], sl, None, ALU.mult, reads=[f"bsw{hh}0", "cst"], writes=[f"bsw{hh}0"])
        P.tt(bsw[hh][1][:], posKf[:], reff[:], ALU.subtract, reads=["posKf", "reff"], writes=[f"bsw{hh}1"])
        P.ts(bsw[hh][1][:], bsw[hh][1][:], sl, None, ALU.mult, reads=[f"bsw{hh}1", "cst"], writes=[f"bsw{hh}1"])
    P.tt(t1[0:64, 0:32], lqk[0:64, 0:32], lqk[0:64, 32:64], ALU.mult, reads=["lqk"], writes=["t1"])
    P.op("dve", lambda e: e.tensor_reduce(small[0:64, 0:1], t1[0:64, 0:32], AX.X, ALU.add), reads=["t1"], writes=["small"])
    P.tt(t1[0:64, 0:32], lqk[0:64, 64:96], lqk[0:64, 96:128], ALU.mult, reads=["lqk", "t1", "small"], writes=["t1"])
    P.op("dve", lambda e: e.tensor_reduce(small[0:64, 1:2], t1[0:64, 0:32], AX.X, ALU.add), reads=["t1"], writes=["small"])
    P.act(small[0:64, 2:4], small[0:64, 0:2], AF.Exp, reads=["small"], writes=["small"])
    P.tt(small[0:64, 4:5], small[0:64, 3:4], small[0:64, 2:3], ALU.subtract, reads=["small"], writes=["small"])
    P.tt(small[0:64, 5:6], small[0:64, 4:5], cst[0:64, 7:8], ALU.subtract, reads=["small", "cst"], writes=["small"])
    neglam = small[0:64, 5:6]

    si = [0]

    def load_cast(dst, src, rows, cols, scale_ap=None, key=None):
        i = si[0] % 2
        si[0] += 1
        P.dma("sp", stg[i][0:rows, 0:cols], src, f"stg{i}", writes=[f"stg{i}"])
        if scale_ap is None:
            P.cp(dst, stg[i][0:rows, 0:cols], reads=[f"stg{i}"], writes=[key], eng="pool" if si[0] % 2 else "dve")
        else:
            P.ts(dst, stg[i][0:rows, 0:cols], scale_ap, None, ALU.mult, reads=[f"stg{i}", "qn", "kvn"], writes=[key])

    for dc in range(8):
        load_cast(wT[:, dc, :], wT_d[dc * 128:(dc + 1) * 128, :], 128, NT, key="wT")
        load_cast(wV[:, dc, :], wV_d[dc * 128:(dc + 1) * 128, :], 128, 192, key="wV")
    for rc in range(2):
        load_cast(wuq[:, rc, :], wuq_d[rc * 128:(rc + 1) * 128, :], 128, 192, scale_ap=qn[:, rc:rc + 1], key="wuq")
    load_cast(wukv[:, :], wukv_d[:, :], 128, 128, scale_ap=kvn[:, 0:1], key="wukv")

    xcnt = [0]

    for c in range(NCH):
        t0 = c * 512
        cb = c % 2
        X = xbf[cb]
        xk = f"xbf{cb}"
        for dc in range(8):
            i = xcnt[0] % 2
            xcnt[0] += 1
            P.dma("sp", xs[i][:], xT_d[dc * 128:(dc + 1) * 128, t0:t0 + 512], f"xs{i}", writes=[f"xs{i}"])
            P.cp(X[:, dc, :], xs[i][:], reads=[f"xs{i}"], writes=[xk], eng="pool" if dc % 2 else "dve")
        P.dma("sp", rp_i[64:96, :], pos32_d[:, t0:t0 + 512], "rp", writes=["rp_i"])
        P.cp(rp_a[64:96, :], rp_i[64:96, :], reads=["rp_i"], writes=["rp_a"])
        P.ts(rp_a[64:96, :], rp_a[64:96, :], cst[64:96, 0:1], None, ALU.mult, reads=["rp_a", "cst"], writes=["rp_a"])
        P.ts(rp_b[64:96, :], rp_a[64:96, :], 1.0 / (2 * math.pi), MAGIC, ALU.mult, op1=ALU.add, reads=["rp_a"], writes=["rp_b"])
        P.ts(rp_b[64:96, :], rp_b[64:96, :], MAGIC, None, ALU.subtract, reads=["rp_b"], writes=["rp_b"])
        P.stt(rp_a[64:96, :], rp_b[64:96, :], -C1, rp_a[64:96, :], ALU.mult, ALU.add, reads=["rp_a", "rp_b"], writes=["rp_a"])
        P.stt(rp_a[64:96, :], rp_b[64:96, :], -C2, rp_a[64:96, :], ALU.mult, ALU.add, reads=["rp_a", "rp_b"], writes=["rp_a"])
        P.ts(rp_a[64:96, :], rp_a[64:96, :], math.pi, -math.pi, ALU.min, op1=ALU.max, reads=["rp_a"], writes=["rp_a"])
        P.act(sinT[64:96, :], rp_a[64:96, :], AF.Sin, scale=cst[64:96, 1:2], reads=["rp_a", "cst"], writes=["sinT"])
        P.act(rp_b[64:96, :], rp_a[64:96, :], AF.Abs, reads=["rp_a", "rp_b"], writes=["rp_b"])
        P.act(cosT[64:96, :], rp_b[64:96, :], AF.Sin, scale=-1.0, bias=cst[64:96, 11:12], reads=["rp_b", "cst"], writes=["cosT"])
        P.dma("sp", rowi[64:65, :], posrow_d[:, t0:t0 + 512], "rw1", writes=["rowi"])
        P.dma("sp", rowi2[64:65, :], refrow_d[:, t0:t0 + 512], "rw2", writes=["rowi2"])
        P.tt(rowf[64:65, :], rowi[64:65, :], rowi2[64:65, :], ALU.subtract, reads=["rowi", "rowi2"], writes=["rowf"])
        for hh in range(2):
            P.act(sinkrow[hh][64:65, :], rowf[64:65, :], AF.Exp, scale=cst[64:65, 3 + hh:4 + hh], bias=cst[64:65, 9 + hh:10 + hh],
                  reads=["rowf", "cst"], writes=[f"sinkrow{hh}"])

        def proj(gname, bank):
            off, M = G[gname]
            for dc in range(8):
                P.mm(B[bank][0:M, :], wT[:, dc, off:off + M], X[:, dc, :], start=(dc == 0), stop=(dc == 7),
                     reads=["wT", xk], writes=[f"B{bank}"])
            return M

        proj("cq0", 0)
        P.cp(cqb[:, 0, :], B[0][:, :], reads=["B0"], writes=["cqb"])
        P.act(sqq[:, 0, :], B[0][:, :], AF.Square, reads=["B0"], writes=["sqq"])
        proj("cq1", 1)
        P.cp(cqb[:, 1, :], B[1][:, :], reads=["B1"], writes=["cqb"])
        P.act(sqq[:, 1, :], B[1][:, :], AF.Square, reads=["B1"], writes=["sqq"])
        proj("ckv", 2)
        P.cp(ckvb[:, :], B[2][:, :], reads=["B2"], writes=["ckvb"])
        P.act(sqkv[:, :], B[2][:, :], AF.Square, reads=["B2"], writes=["sqkv"])
        P.mm(B[6][:, :], onesb[:, :], sqq[:, 0, :], start=True, stop=False, reads=["onesb", "sqq"], writes=["B6"])
        P.mm(B[6][:, :], onesb[:, :], sqq[:, 1, :], start=False, stop=True, reads=["onesb", "sqq"], writes=["B6"])
        P.act(rq[:], B[6][:, :], AF.Sqrt, scale=1.0 / 256, bias=cst[:, 12:13], reads=["B6", "cst"], writes=["rq"])
        P.recip(rq[:], rq[:], reads=["rq"], writes=["rq"])
        P.mm(B[7][:, :], onesb[:, :], sqkv[:, :], reads=["onesb", "sqkv"], writes=["B7"])
        P.act(rkv[:], B[7][:, :], AF.Sqrt, scale=1.0 / 128, bias=cst[:, 12:13], reads=["B7", "cst"], writes=["rkv"])
        P.recip(rkv[:], rkv[:], reads=["rkv"], writes=["rkv"])
        proj("kr", 3)
        proj("krs", 4)
        P.tt(t1[64:96, :], B[3][64:96, :], cosT[64:96, :], ALU.mult, reads=["B3", "cosT"], writes=["t1"])
        P.tt(t2[64:96, :], B[4][64:96, :], sinT[64:96, :], ALU.mult, reads=["B4", "sinT"], writes=["t2"])
        P.tt(KA[64:96, t0:t0 + 512], t1[64:96, :], t2[64:96, :], ALU.add, reads=["t1", "t2"], writes=[f"KA{c}"])
        for rc in range(2):
            P.mm(B[0][0:96, :], wuq[:, rc, 0:96], cqb[:, rc, :], start=(rc == 0), stop=(rc == 1), reads=["wuq", "cqb"], writes=["B0"])
        for rc in range(2):
            P.mm(B[1][0:96, :], wuq[:, rc, 96:192], cqb[:, rc, :], start=(rc == 0), stop=(rc == 1), reads=["wuq", "cqb"], writes=["B1"])
        qak = f"QA{cb}"
        P.tt(QA[cb][0:64, :], B[0][0:64, :], rq[0:64, :], ALU.mult, reads=["B0", "rq"], writes=[qak])
        P.tt(t1[64:96, :], B[0][64:96, :], cosT[64:96, :], ALU.mult, reads=["B0", "cosT", "t1"], writes=["t1"])
        P.tt(t2[64:96, :], B[1][64:96, :], sinT[64:96, :], ALU.mult, reads=["B1", "sinT", "t2"], writes=["t2"])
        P.tt(t1[64:96, :], t1[64:96, :], t2[64:96, :], ALU.add, reads=["t1", "t2"], writes=["t1"])
        P.tt(QA[cb][64:96, :], t1[64:96, :], rq[64:96, :], ALU.mult, reads=["t1", "rq"], writes=[qak])
        P.mm(B[2][0:64, :], wukv[:, 0:64], ckvb[:, :], reads=["wukv", "ckvb"], writes=["B2"])
        P.tt(KA[0:64, t0:t0 + 512], B[2][0:64, :], rkv[0:64, :], ALU.mult, reads=["B2", "rkv"], writes=[f"KA{c}"])
        proj("qB", 3)
        P.cp(QB[cb][0:96, :], B[3][0:96, :], reads=["B3"], writes=[f"QB{cb}"], eng="act")
        proj("kB", 4)
        P.cp(KB[0:96, t0:t0 + 512], B[4][0:96, :], reads=["B4"], writes=[f"KB{c}"])
        proj("qC", 0)
        P.cp(QC[cb][0:32, :], B[0][0:32, :], reads=["B0"], writes=[f"QC{cb}"], eng="act")
        proj("kC", 1)
        P.cp(KC[0:32, t0:t0 + 512], B[1][0:32, :], reads=["B1"], writes=[f"KC{c}"])
        proj("swqA", 2)
        P.cp(QSW[cb][0:64, 0, :], B[2][0:64, :], reads=["B2"], writes=[f"QSW{cb}"], eng="act")
        proj("swqB", 3)
        P.cp(QSW[cb][0:64, 1, :], B[3][0:64, :], reads=["B3"], writes=[f"QSW{cb}"])
        proj("swk", 4)
        P.cp(KSW[0:64, cb, :], B[4][0:64, :], reads=["B4"], writes=[f"KSW{cb}"], eng="act")
        for j in range(4):
            blk = 4 * c + j
            for dc in range(8):
                P.mm(B[5][:, 0:192], X[:, dc, j * 128:(j + 1) * 128], wV[:, dc, :], start=(dc == 0), stop=(dc == 7),
                     reads=[xk, "wV"], writes=["B5"])
            P.cp(Vsb[:, blk, 0:64], B[5][:, 0:64], reads=["B5"], writes=[f"Vsb{blk}"])
            P.cp(Vsw[:, blk % 8, 0:64], B[5][:, 64:128], reads=["B5"], writes=[f"Vsw{blk % 8}"], eng="act")
            P.cp(Vdf[:, blk, 0:64], B[5][:, 128:192], reads=["B5"], writes=[f"Vdf{blk}"])
            P.mm(B[6][:, 0:64], ckvb[:, j * 128:(j + 1) * 128], wukv[:, 64:128], reads=["ckvb", "wukv"], writes=["B6"])
            P.mm(B[7][:, 0:1], sqkv[:, j * 128:(j + 1) * 128], onesb[:, 0:1], reads=["sqkv", "onesb"], writes=["B7"])
            P.act(rstok[:, j:j + 1], B[7][:, 0:1], AF.Sqrt, scale=1.0 / 128, bias=cst[:, 12:13], reads=["B7", "cst"], writes=["rstok"])
            P.recip(rstok[:, j:j + 1], rstok[:, j:j + 1], reads=["rstok"], writes=["rstok"])
            P.ts(Va[:, blk, 0:64], B[6][:, 0:64], rstok[:, j:j + 1], None, ALU.mult, reads=["B6", "rstok"], writes=[f"Va{blk}"])

        nkb = 4 * c + 4
        oi = [0]

        def finalize_norm(obank, rows_out, extra_den=None, post=None):
            bk = f"B{obank}"
            if extra_den is None:
                P.cp(dn[64:65, :], B[obank][64:65, :], reads=[bk], writes=["dn"])
            else:
                ap, key = extra_den
                P.tt(dn[64:65, :], B[obank][64:65, :], ap, ALU.add, reads=[bk, key], writes=["dn"])
            P.recip(dn[64:65, :], dn[64:65, :], reads=["dn"], writes=["dn"])
            P.mm(B[6][0:64, :], onesf[64:65, 0:64], dn[64:65, :], reads=["onesf", "dn"], writes=["B6"])
            P.cp(bc[:, :], B[6][0:64, :], reads=["B6"], writes=["bc"], eng="act")
            return bc

        def store(src_ap, key, row0):
            P.dma("pool", oT_d[row0:row0 + 64, t0:t0 + 512], src_ap, "st_" + key, reads=[key])

        sc_mla = 96.0 ** -0.5

        def mla_qk(i):
            P.mm(B[i % 4][:, :], KA[0:96, i * 128:(i + 1) * 128], QA[cb][0:96, :], reads=[f"KA{i // 4}", qak], writes=[f"B{i % 4}"])

        def mla_exp(i):
            p = i % 3
            P.act(Pt[p][:], B[i % 4][:, :], AF.Exp, scale=sc_mla, reads=[f"B{i % 4}"], writes=[f"Pt{p}"])
            if i >= 4 * c:
                P.tt(Pt[p][:], Pt[p][:], mI[i - 4 * c][:], ALU.mult, reads=[f"Pt{p}", f"mI{i - 4 * c}"], writes=[f"Pt{p}"])

        def mla_pv(i):
            p = i % 3
            P.mm(B[4][0:65, :], Va[:, i, 0:65], Pt[p][:], start=(i == 0), stop=(i == nkb - 1),
                 reads=[f"Va{i}", "Va1", f"Pt{p}"], writes=["B4"])

        pipeline(nkb, [(2, mla_qk), (0, mla_exp), (0, mla_pv)])
        finalize_norm(4, 0)
        o = ob[oi[0] % 3]; ok = f"ob{oi[0] % 3}"; oi[0] += 1
        P.tt(o[:, :], B[4][0:64, :], bc[:, :], ALU.mult, reads=["B4", "bc"], writes=[ok])
        store(o[:, :], ok, 0)

        sc_df = 32.0 ** -0.5
        P.ts(btab[:, 0:nkb], posKf[:, 0:nkb], pmidf[:, c:c + 1], cst[:, 2:3], ALU.subtract, op1=ALU.mult,
             reads=["posKf", "pmidf", "cst"], writes=["btab"])
        for mp in range(2):
            if mp == 0:
                Kt, Qt, kkey, qkey, p0, p1 = KB, QB[cb], "KB", f"QB{cb}", 64, 96
            else:
                Kt, Qt, kkey, qkey, p0, p1 = KC, QC[cb], "KC", f"QC{cb}", 0, 32
            obank = 4 + mp

            def df_qk(i, Kt=Kt, Qt=Qt, kkey=kkey, qkey=qkey, p0=p0, p1=p1):
                P.mm(B[i % 4][:, :], Kt[p0:p1, i * 128:(i + 1) * 128], Qt[p0:p1, :], reads=[f"{kkey}{i // 4}", qkey], writes=[f"B{i % 4}"])

            def df_exp(i):
                p = i % 3
                P.act(Pt[p][:], B[i % 4][:, :], AF.Exp, scale=sc_df, bias=btab[:, i:i + 1], reads=[f"B{i % 4}", "btab"], writes=[f"Pt{p}"])
                if i >= 4 * c:
                    P.tt(Pt[p][:], Pt[p][:], mI[i - 4 * c][:], ALU.mult, reads=[f"Pt{p}", f"mI{i - 4 * c}"], writes=[f"Pt{p}"])

            def df_pv(i, obank=obank):
                p = i % 3
                P.mm(B[obank][0:65, :], Vdf[:, i, 0:65], Pt[p][:], start=(i == 0), stop=(i == nkb - 1),
                     reads=[f"Vdf{i}", "Vdf1", f"Pt{p}"], writes=[f"B{obank}"])

            pipeline(nkb, [(2, df_qk), (0, df_exp), (0, df_pv)])
        finalize_norm(4, 0)
        P.tt(fa[:, :], B[4][0:64, :], bc[:, :], ALU.mult, reads=["B4", "bc"], writes=["fa"])
        finalize_norm(5, 0)
        P.tt(fb[:, :], B[5][0:64, :], bc[:, :], ALU.mult, reads=["B5", "bc"], writes=["fb"])
        P.stt(fa[:, :], fb[:, :], neglam, fa[:, :], ALU.mult, ALU.add, reads=["fa", "fb", "small"], writes=["fa"])
        P.act(fsq[:, :], fa[:, :], AF.Square, reads=["fa"], writes=["fsq"])
        P.mm(B[6][0:64, :], onesb[0:64, 0:64], fsq[:, :], reads=["onesb", "fsq"], writes=["B6"])
        P.act(fb[:, :], B[6][0:64, :], AF.Sqrt, scale=1.0 / 64, bias=cst[0:64, 12:13], reads=["B6", "cst", "fb"], writes=["fb"])
        P.recip(fb[:, :], fb[:, :], reads=["fb"], writes=["fb"])
        P.tt(fa[:, :], fa[:, :], fb[:, :], ALU.mult, reads=["fa", "fb"], writes=["fa"])
        o = ob[oi[0] % 3]; ok = f"ob{oi[0] % 3}"; oi[0] += 1
        P.ts(o[:, :], fa[:, :], cst[0:64, 13:14], cst[0:64, 8:9], ALU.mult, op1=ALU.mult, reads=["fa", "cst"], writes=[ok])
        store(o[:, :], ok, 256)

        order = list(range(nkb - 1, -1, -1))
        qbk = f"QB{cb}"

        def sb_qk(i):
            kb = order[i]
            P.mm(B[i % 4][:, :], KB[0:64, kb * 128:(kb + 1) * 128], QB[cb][0:64, :], start=True, stop=False,
                 reads=[f"KB{kb // 4}", qbk], writes=[f"B{i % 4}"])

        def sb_sp(i):
            kb = order[i]
            e = i % 2
            P.act(ef[0][:], B[i % 4][:, :], AF.Exp, scale=0.125, reads=[f"B{i % 4}"], writes=["ef0"])
            P.act(spb[e][:], ef[0][:], AF.Ln, bias=1.0, reads=["ef0"], writes=[f"spb{e}"])
            if kb >= 4 * c:
                P.tt(spb[e][:], spb[e][:], mS[kb - 4 * c][:], ALU.mult, reads=[f"spb{e}", f"mS{kb - 4 * c}"], writes=[f"spb{e}"])

        def sb_A(i):
            kb = order[i]
            e = i % 2
            bk = i % 4
            last_is_R = i > 0
            P.mm(B[bk][:, :], Um[:, :], spb[e][:], start=False, stop=not last_is_R, reads=["Um", f"spb{e}"], writes=[f"B{bk}"])
            if last_is_R:
                P.mm(B[bk][:, :], On8[:, :], Rb[:], start=False, stop=True, reads=["On8", "Rb"], writes=[f"B{bk}"])
            if i == 0:
                P.cp(Rb[:], spb[e][:], reads=[f"spb{e}", "Rb"], writes=["Rb"], eng="pool")
            else:
                P.tt(Rb[:], Rb[:], spb[e][:], ALU.add, reads=[f"spb{e}", "Rb"], writes=["Rb"], eng="pool")

        def sb_exp2(i):
            kb = order[i]
            p = i % 3
            bk = i % 4
            P.act(Pt[p][:], B[bk][:, :], AF.Exp, scale=0.125, reads=[f"B{bk}"], writes=[f"Pt{p}"])
            if kb >= 4 * c:
                P.tt(Pt[p][:], Pt[p][:], mS[kb - 4 * c][:], ALU.mult, reads=[f"Pt{p}", f"mS{kb - 4 * c}"], writes=[f"Pt{p}"])

        def sb_pv(i):
            kb = order[i]
            p = i % 3
            P.mm(B[4][0:64, :], Vsb[:, kb, 0:64], Pt[p][:], start=(i == 0), stop=(i == nkb - 1),
                 reads=[f"Vsb{kb}", f"Pt{p}"], writes=["B4"])

        pipeline(nkb, [(2, sb_qk), (1, sb_sp), (1, sb_A), (0, sb_exp2), (0, sb_pv)])
        o = ob[oi[0] % 3]; ok = f"ob{oi[0] % 3}"; oi[0] += 1
        P.cp(o[:, :], B[4][0:64, :], reads=["B4"], writes=[ok], eng="act")
        store(o[:, :], ok, 64)

        sc_sw = 64.0 ** -0.5
        qsk = f"QSW{cb}"
        it = 0
        for jn in range(4):
            n = 4 * c + jn
            kbs = [kb for kb in (n - 1, n) if kb >= 0]
            for ki, kb in enumerate(kbs):
                which = 0 if kb == n - 1 else 1
                sbk = it % 2
                p = it % 3
                it += 1
                kslot = (kb // 4) % 2
                kof = (kb % 4) * 128
                for hh in range(2):
                    P.mm(B[sbk][:, hh * 128:(hh + 1) * 128], KSW[0:64, kslot, kof:kof + 128], QSW[cb][0:64, hh, jn * 128:(jn + 1) * 128],
                         reads=[f"KSW{kslot}", qsk], writes=[f"B{sbk}"])
                for hh in range(2):
                    P.act(Pt[p][:, hh * 128:(hh + 1) * 128], B[sbk][:, hh * 128:(hh + 1) * 128], AF.Exp, scale=sc_sw,
                          bias=bsw[hh][which][:, n:n + 1], reads=[f"B{sbk}", f"bsw{hh}{which}"], writes=[f"Pt{p}"])
                P.tt(Pt[p][:, 0:256], Pt[p][:, 0:256], mSW[which][:, :], ALU.mult, reads=[f"Pt{p}", f"mSW{which}"], writes=[f"Pt{p}"])
                for hh in range(2):
                    P.mm(B[4 + hh][0:65, jn * 128:(jn + 1) * 128], Vsw[:, kb % 8, 0:65], Pt[p][:, hh * 128:(hh + 1) * 128],
                         start=(ki == 0), stop=(ki == len(kbs) - 1), reads=[f"Vsw{kb % 8}", "Vsw1", f"Pt{p}"], writes=[f"B{4 + hh}"])
        for hh in range(2):
            finalize_norm(4 + hh, 0, extra_den=(sinkrow[hh][64:65, :], f"sinkrow{hh}"))
            o = ob[oi[0] % 3]; ok = f"ob{oi[0] % 3}"; oi[0] += 1
            P.tt(o[:, :], B[4 + hh][0:64, :], bc[:, :], ALU.mult, reads=[f"B{4 + hh}", "bc"], writes=[ok])
            store(o[:, :], ok, 128 + 64 * hh)

    P.emit()
    return nc

ALPHA = 8.0 ** 0.25
TOK = 2048
NG = TOK // 512
BR_CH = (2, 2, 4, 2)


def build_B1():
    nc = bass.Bass("TRN2", target_bir_lowering=False)
    P = Prog(nc)

    def din(name, shape, dt=F32):
        return nc.dram_tensor(name, list(shape), dt, kind="ExternalInput").ap()

    def dout(name, shape, dt=F32):
        return nc.dram_tensor(name, list(shape), dt, kind="ExternalOutput").ap()

    xT_d = din("xT", [1024, TOK])
    x_d = din("x", [TOK, 1024])
    oT_d = din("oT", [1280, TOK], BF16)
    wg_d = din("w_gate", [1024, 4096])
    bg_d = din("bgT", [128, 32])
    wbr_d = din("w_br", [1280, 1024])
    wo_d = din("w_out", [1024, 1024])
    lng_d = din("lng", [128, 1024])
    lnb_d = din("lnb", [128, 1024])
    wr_d = din("w_router", [1024, 32])
    rb_d = din("rbias", [128, 32])
    y1_d = dout("y1", [TOK, 1024])
    x2T_d = dout("x2T", [1024, TOK], BF16)
    gw_d = dout("gw", [TOK, 32])

    wg = P.sb([128, 8, 4096], BF16, "wg")
    wbr = P.sb([128, 10, 1024], BF16, "wbr")
    wo = P.sb([128, 8, 1024], BF16, "wo")
    stg = [P.sb([128, 1024], F32, f"stg{i}") for i in range(2)]
    bg = P.sb([128, 32], F32, "bg")
    lng = P.sb([128, 1024], F32, "lng")
    lnb = P.sb([128, 1024], F32, "lnb")
    wr = P.sb([128, 8, 32], F32, "wr")
    rb = P.sb([128, 32], F32, "rb")
    ioi = P.sb([128, 128], I32, "ioi")
    iof = P.sb([128, 128], F32, "iof")
    ident = P.sb([128, 128], F32, "ident")
    epsc = P.sb([128, 1], F32, "epsc")
    xs = [P.sb([128, 512], F32, f"xs{i}") for i in range(2)]
    xg = [P.sb([128, 8, 512], BF16, f"xg{i}") for i in range(2)]
    og = P.sb([128, 10, 512], BF16, "og")
    mT = P.sb([128, 8, 512], BF16, "mT")
    gs = [P.sb([128, 512], F32, f"gs{i}") for i in range(2)]
    macc = P.sb([128, 512], F32, "macc")
    mtmp = P.sb([128, 512], F32, "mtmp")
    xt = [P.sb([128, 1024], F32, f"xt{i}") for i in range(2)]
    yb = P.sb([128, 1024], F32, "yb")
    x2 = P.sb([128, 1024], F32, "x2")
    y1 = [P.sb([128, 1024], F32, f"y1_{i}") for i in range(2)]
    x2Tf = P.sb([128, 8, 128], F32, "x2Tf")
    x2Tb = [P.sb([128, 8, 128], BF16, f"x2Tb{i}") for i in range(2)]
    st = P.sb([128, 16], F32, "st")
    sc = P.sb([128, 32], F32, "sc")
    bi = P.sb([128, 32], F32, "bi")
    pr = P.sb([128, 6, 8], F32, "pr")
    pm = P.sb([128, 6, 8], F32, "pm")
    gsum = P.sb([128, 8], F32, "gsum")
    thr = P.sb([128, 8], F32, "thr")
    gsel = P.sb([128, 8], F32, "gsel")
    em = P.sb([128, 32], F32, "em")
    gwt = [P.sb([128, 32], F32, f"gwt{i}") for i in range(2)]
    B = [P.ps([128, 512], F32, f"bank{i}") for i in range(8)]
    P.excl = set(f"B{i}" for i in range(8))

    for i, (t, d_, k) in enumerate(((bg, bg_d, "bg"), (lng, lng_d, "lng"), (lnb, lnb_d, "lnb"), (rb, rb_d, "rb"))):
        P.dma("sp", t[:], d_[:, :], f"c{i}", writes=[k])
    P.dma("sp", wr[:], wr_d.rearrange("(dc p) e -> p dc e", p=128), "c9", writes=["wr"])
    P.op("pool", lambda e: e.iota(ioi[:], [[1, 128]], base=0, channel_multiplier=-1), writes=["ioi"])
    P.cp(iof[:], ioi[:], reads=["ioi"], writes=["iof"])
    P.ts(ident[:], iof[:], 0.0, None, ALU.is_equal, reads=["iof"], writes=["ident"])
    P.memset(epsc[:], LN_EPS, writes=["epsc"])
    si = [0]

    def load_cast(dst, src, cols, key):
        i = si[0] % 2
        si[0] += 1
        P.dma("sp", stg[i][:, 0:cols], src, f"stg{i}", writes=[f"stg{i}"])
        P.cp(dst, stg[i][:, 0:cols], reads=[f"stg{i}"], writes=[key], eng="pool" if si[0] % 2 else "dve")

    for dc in range(8):
        for q in range(4):
            load_cast(wg[:, dc, q * 1024:(q + 1) * 1024], wg_d[dc * 128:(dc + 1) * 128, q * 1024:(q + 1) * 1024], 1024, "wg")
    for ch in range(10):
        load_cast(wbr[:, ch, :], wbr_d[ch * 128:(ch + 1) * 128, :], 1024, "wbr")
    for dc in range(8):
        load_cast(wo[:, dc, :], wo_d[dc * 128:(dc + 1) * 128, :], 1024, "wo")

    br_off = [0, 2, 4, 8]
    xcnt = [0]
    bk = [0]
    tcount = [0]
    for g in range(NG):
        t0 = g * 512
        X = xg[g % 2]
        xk = f"xg{g % 2}"
        for dc in range(8):
            i = xcnt[0] % 2
            xcnt[0] += 1
            P.dma("sp", xs[i][:], xT_d[dc * 128:(dc + 1) * 128, t0:t0 + 512], f"xs{i}", writes=[f"xs{i}"])
            P.cp(X[:, dc, :], xs[i][:], reads=[f"xs{i}"], writes=[xk], eng="pool" if dc % 2 else "dve")
        P.dma("sp", og[:], oT_d[:, t0:t0 + 512].rearrange("(ch p) t -> p ch t", p=128), "og", writes=["og"])
        for fc in range(8):
            for i in range(4):
                gb = bk[0] % 2
                rbk = 2 + bk[0] % 2
                bk[0] += 1
                col = i * 1024 + fc * 128
                for dc in range(8):
                    P.mm(B[gb][:, :], wg[:, dc, col:col + 128], X[:, dc, :], start=(dc == 0), stop=(dc == 7),
                         reads=["wg", xk], writes=[f"B{gb}"])
                gi = bk[0] % 2
                P.act(gs[gi][:], B[gb][:, :], AF.Sigmoid, bias=bg[:, i * 8 + fc:i * 8 + fc + 1], reads=[f"B{gb}", "bg"], writes=[f"gs{gi}"])
                n = BR_CH[i]
                for k in range(n):
                    ch = br_off[i] + k
                    P.mm(B[rbk][:, :], wbr[:, ch, fc * 128:(fc + 1) * 128], og[:, ch, :], start=(k == 0), stop=(k == n - 1),
                         reads=["wbr", "og"], writes=[f"B{rbk}"])
                if i == 0:
                    P.tt(macc[:], gs[gi][:], B[rbk][:, :], ALU.mult, reads=[f"gs{gi}", f"B{rbk}"], writes=["macc"])
                else:
                    P.tt(mtmp[:], gs[gi][:], B[rbk][:, :], ALU.mult, reads=[f"gs{gi}", f"B{rbk}"], writes=["mtmp"])
                    if i < 3:
                        P.tt(macc[:], macc[:], mtmp[:], ALU.add, reads=["macc", "mtmp"], writes=["macc"], eng="pool")
                    else:
                        P.tt(mT[:, fc, :], macc[:], mtmp[:], ALU.add, reads=["macc", "mtmp"], writes=["mT"], eng="pool")
        for j in range(4):
            tt_ = tcount[0]
            tcount[0] += 1
            tok0 = t0 + j * 128
            xb = xt[tt_ % 2]
            xbk = f"xt{tt_ % 2}"
            P.dma("sp", xb[:], x_d[tok0:tok0 + 128, :], xbk, writes=[xbk])
            for ct in range(2):
                bnk = 4 + ct
                for fc in range(8):
                    P.mm(B[bnk][:, :], mT[:, fc, j * 128:(j + 1) * 128], wo[:, fc, ct * 512:(ct + 1) * 512], start=(fc == 0), stop=(fc == 7),
                         reads=["mT", "wo"], writes=[f"B{bnk}"])
                P.stt(yb[:, ct * 512:(ct + 1) * 512], xb[:, ct * 512:(ct + 1) * 512], ALPHA, B[bnk][:, :], ALU.mult, ALU.add,
                      reads=[xbk, f"B{bnk}"], writes=["yb"])
            P.op("dve", lambda e: e.bn_stats(st[:, 0:6], yb[:, 0:512]), reads=["yb"], writes=["st"])
            P.op("dve", lambda e: e.bn_stats(st[:, 6:12], yb[:, 512:1024]), reads=["yb"], writes=["st"])
            P.op("dve", lambda e: e.bn_aggr(st[:, 12:14], st[:, 0:12]), reads=["st"], writes=["st"])
            P.act(st[:, 14:15], st[:, 13:14], AF.Sqrt, bias=epsc[:, 0:1], reads=["st", "epsc"], writes=["st"])
            P.recip(st[:, 15:16], st[:, 14:15], reads=["st"], writes=["st"])
            P.ts(x2[:], yb[:], st[:, 12:13], st[:, 15:16], ALU.subtract, op1=ALU.mult, reads=["yb", "st"], writes=["x2"])
            P.tt(x2[:], x2[:], lng[:], ALU.mult, reads=["x2", "lng"], writes=["x2"], eng="pool")
            P.tt(x2[:], x2[:], lnb[:], ALU.add, reads=["x2", "lnb"], writes=["x2"])
            yo = y1[tt_ % 2]
            yk = f"y1_{tt_ % 2}"
            P.ts(yo[:], x2[:], ALPHA, None, ALU.mult, reads=["x2"], writes=[yk], eng="pool")
            P.dma("pool", y1_d[tok0:tok0 + 128, :], yo[:], "st_" + yk, reads=[yk])
            for dc in range(8):
                bnk = 6 + dc // 4
                P.tr(B[bnk][:, (dc % 4) * 128:(dc % 4 + 1) * 128], x2[:, dc * 128:(dc + 1) * 128], ident[:],
                     reads=["x2", "ident"], writes=[f"B{bnk}"])
            xtb = x2Tb[tt_ % 2]
            xtk = f"x2Tb{tt_ % 2}"
            for hb in range(2):
                P.cp(x2Tf[:, hb * 4:(hb + 1) * 4, :], B[6 + hb][:, :], reads=[f"B{6 + hb}"], writes=["x2Tf"], eng="act")
                P.cp(xtb[:, hb * 4:(hb + 1) * 4, :], B[6 + hb][:, :], reads=[f"B{6 + hb}"], writes=[xtk])
            P.dma("pool", x2T_d[:, tok0:tok0 + 128].rearrange("(dc p) t -> p dc t", p=128), xtb[:], "st_" + xtk, reads=[xtk])
            for dc in range(8):
                P.mm(B[6][:, 0:32], x2Tf[:, dc, :], wr[:, dc, :], start=(dc == 0), stop=(dc == 7), reads=["x2Tf", "wr"], writes=["B6"])
            P.act(sc[:], B[6][:, 0:32], AF.Sigmoid, reads=["B6"], writes=["sc"])
            P.tt(bi[:], sc[:], rb[:], ALU.add, reads=["sc", "rb"], writes=["bi"])
            v = bi[:, :].rearrange("p (g e) -> p g e", e=4)
            pairs = ((0, 1), (0, 2), (0, 3), (1, 2), (1, 3), (2, 3))
            for k, (a, b_) in enumerate(pairs):
                P.tt(pr[:, k, :], v[:, :, a], v[:, :, b_], ALU.add, reads=["bi"], writes=["pr"])
                P.tt(pm[:, k, :], v[:, :, a], v[:, :, b_], ALU.min, reads=["bi"], writes=["pm"])
            P.tt(gsum[:], pr[:, 0, :], pr[:, 1, :], ALU.max, reads=["pr"], writes=["gsum"])
            P.tt(thr[:], pm[:, 0, :], pm[:, 1, :], ALU.max, reads=["pm"], writes=["thr"])
            for k in range(2, 6):
                P.tt(gsum[:], gsum[:], pr[:, k, :], ALU.max, reads=["pr", "gsum"], writes=["gsum"])
                P.tt(thr[:], thr[:], pm[:, k, :], ALU.max, reads=["pm", "thr"], writes=["thr"])
            P.op("dve", lambda e: e.tensor_reduce(st[:, 0:1], gsum[:], AX.X, ALU.max), reads=["gsum", "st"], writes=["st"])
            P.ts(gsel[:], gsum[:], st[:, 0:1], None, ALU.is_ge, reads=["gsum", "st"], writes=["gsel"])
            emv = em[:, :].rearrange("p (g e) -> p g e", e=4)
            for a in range(4):
                P.tt(emv[:, :, a], v[:, :, a], thr[:], ALU.is_ge, reads=["bi", "thr"], writes=["em"])
                P.tt(emv[:, :, a], emv[:, :, a], gsel[:], ALU.mult, reads=["em", "gsel"], writes=["em"])
            P.tt(em[:], em[:], sc[:], ALU.mult, reads=["em", "sc"], writes=["em"])
            P.op("dve", lambda e: e.tensor_reduce(st[:, 1:2], em[:], AX.X, ALU.add), reads=["em", "st"], writes=["st"])
            P.recip(st[:, 2:3], st[:, 1:2], reads=["st"], writes=["st"])
            go = gwt[tt_ % 2]
            gk = f"gwt{tt_ % 2}"
            P.ts(go[:], em[:], st[:, 2:3], None, ALU.mult, reads=["em", "st"], writes=[gk])
            P.dma("pool", gw_d[tok0:tok0 + 128, :], go[:], "st_" + gk, reads=[gk])
    P.emit()
    return nc


def build_B2():
    nc = bass.Bass("TRN2", target_bir_lowering=False)
    P = Prog(nc)

    def din(name, shape, dt=F32):
        return nc.dram_tensor(name, list(shape), dt, kind="ExternalInput").ap()

    y1_d = din("y1", [TOK, 1024])
    x2T_d = din("x2T", [1024, TOK], BF16)
    gw_d = din("gw", [TOK, 32])
    w1_d = din("w1", [32, 1024, 256])
    w3_d = din("w3", [32, 1024, 256])
    w2_d = din("w2", [32, 256, 1024])
    lng_d = din("lng", [128, 1024])
    lnb_d = din("lnb", [128, 1024])
    out_d = nc.dram_tensor("xo", [TOK, 1024], F32, kind="ExternalOutput").ap()

    NTL = TOK // 128
    acc = P.sb([128, NTL, 1024], F32, "acc")
    x2T = P.sb([128, 8, TOK], BF16, "x2T")
    gw = P.sb([128, NTL, 32], F32, "gw")
    lng = P.sb([128, 1024], F32, "lng")
    lnb = P.sb([128, 1024], F32, "lnb")
    epsc = P.sb([128, 1], F32, "epsc")
    stg = [P.sb([128, 2048], F32, f"stg{i}") for i in range(3)]
    w13 = [P.sb([128, 8, 512], BF16, f"w13_{i}") for i in range(2)]
    w2 = [P.sb([128, 2, 1024], BF16, f"w2_{i}") for i in range(2)]
    sl = [P.sb([128, 2, 512], F32, f"sl{i}") for i in range(2)]
    hT = [P.sb([128, 2, 512], BF16, f"hT{i}") for i in range(2)]
    st = P.sb([128, 16], F32, "st")
    xo = [P.sb([128, 1024], F32, f"xo{i}") for i in range(2)]
    B = [P.ps([128, 512], F32, f"bank{i}") for i in range(8)]
    P.excl = set(f"B{i}" for i in range(8))

    P.dma("sp", lng[:], lng_d[:, :], "c0", writes=["lng"])
    P.dma("sp", lnb[:], lnb_d[:, :], "c1", writes=["lnb"])
    P.memset(epsc[:], LN_EPS, writes=["epsc"])
    P.dma("sp", x2T[:], x2T_d.rearrange("(dc p) t -> p dc t", p=128), "c2", writes=["x2T"])
    P.dma("sp", gw[:], gw_d.rearrange("(j p) e -> p j e", p=128), "c3", writes=["gw"])
    for j in range(NTL):
        P.dma("sp", acc[:, j, :], y1_d[j * 128:(j + 1) * 128, :], "c4", writes=[f"acc{j}"])

    si = [0]
    ybk = [0]
    for e in range(32):
        eb = e % 2
        wk = f"w13_{eb}"
        w2k = f"w2_{eb}"
        for which, src in enumerate((w1_d, w3_d)):
            i = si[0] % 3
            si[0] += 1
            P.dma("sp", stg[i][:, :].rearrange("p (dc f) -> p dc f", dc=8), src[e].rearrange("(dc p) f -> p dc f", p=128), f"stg{i}", writes=[f"stg{i}"])
            P.cp(w13[eb][:, :, which * 256:(which + 1) * 256], stg[i][:, :].rearrange("p (dc f) -> p dc f", dc=8), reads=[f"stg{i}"], writes=[wk],
                 eng="pool")
        i = si[0] % 3
        si[0] += 1
        P.dma("sp", stg[i][:, :].rearrange("p (fc d) -> p fc d", fc=2), w2_d[e].rearrange("(fc p) d -> p fc d", p=128), f"stg{i}", writes=[f"stg{i}"])
        P.cp(w2[eb][:, :, :], stg[i][:, :].rearrange("p (fc d) -> p fc d", fc=2), reads=[f"stg{i}"], writes=[w2k], eng="pool")
        for g in range(NG):
            t0 = g * 512
            hb = g % 2
            for q in range(4):
                for dc in range(8):
                    P.mm(B[q][:, :], w13[eb][:, dc, q * 128:(q + 1) * 128], x2T[:, dc, t0:t0 + 512], start=(dc == 0), stop=(dc == 7),
                         reads=[wk, "x2T"], writes=[f"B{q}"])
            for fc in range(2):
                P.act(sl[hb][:, fc, :], B[fc][:, :], AF.Silu, reads=[f"B{fc}"], writes=[f"sl{hb}"])
                P.tt(hT[hb][:, fc, :], sl[hb][:, fc, :], B[2 + fc][:, :], ALU.mult, reads=[f"sl{hb}", f"B{2 + fc}"], writes=[f"hT{hb}"])
            for j in range(4):
                tj = g * 4 + j
                for ct in range(2):
                    bnk = 4 + ybk[0] % 4
                    ybk[0] += 1
                    for fc in range(2):
                        P.mm(B[bnk][:, :], hT[hb][:, fc, j * 128:(j + 1) * 128], w2[eb][:, fc, ct * 512:(ct + 1) * 512], start=(fc == 0), stop=(fc == 1),
                             reads=[f"hT{hb}", w2k], writes=[f"B{bnk}"])
                    P.stt(acc[:, tj, ct * 512:(ct + 1) * 512], B[bnk][:, :], gw[:, tj, e:e + 1], acc[:, tj, ct * 512:(ct + 1) * 512], ALU.mult, ALU.add,
                          reads=[f"B{bnk}", "gw", f"acc{tj}"], writes=[f"acc{tj}"])
    for j in range(NTL):
        a = acc[:, j, :]
        ak = f"acc{j}"
        P.op("dve", lambda e, a=a: e.bn_stats(st[:, 0:6], a[:, 0:512]), reads=[ak, "st"], writes=["st"])
        P.op("dve", lambda e, a=a: e.bn_stats(st[:, 6:12], a[:, 512:1024]), reads=[ak], writes=["st"])
        P.op("dve", lambda e: e.bn_aggr(st[:, 12:14], st[:, 0:12]), reads=["st"], writes=["st"])
        P.act(st[:, 14:15], st[:, 13:14], AF.Sqrt, bias=epsc[:, 0:1], reads=["st", "epsc"], writes=["st"])
        P.recip(st[:, 15:16], st[:, 14:15], reads=["st"], writes=["st"])
        o = xo[j % 2]
        ok = f"xo{j % 2}"
        P.ts(o[:], a, st[:, 12:13], st[:, 15:16], ALU.subtract, op1=ALU.mult, reads=[ak, "st"], writes=[ok])
        P.tt(o[:], o[:], lng[:], ALU.mult, reads=[ok, "lng"], writes=[ok], eng="pool")
        P.tt(o[:], o[:], lnb[:], ALU.add, reads=[ok, "lnb"], writes=[ok])
        P.dma("pool", out_d[j * 128:(j + 1) * 128, :], o[:], "st_" + ok, reads=[ok])
    P.emit()
    return nc


def alibi(n):
    return (2.0 ** (-8.0 * np.arange(1, n + 1, dtype=np.float32) / n)).astype(np.float32)


def prepA_shared(inp, S):
    pos = np.ascontiguousarray(inp["positions"][:S]).astype(np.int32)
    NB = S // 128
    NCH = S // 512
    d = {}
    d["posK"] = np.ascontiguousarray(pos.reshape(NB, 128).T)
    d["pmidB"] = np.ascontiguousarray(np.broadcast_to(pos[256::512][None, :], (128, NCH)))
    d["refB"] = np.ascontiguousarray(np.broadcast_to(pos[0::128][None, :], (128, NB)))
    d["pos32"] = np.ascontiguousarray(np.broadcast_to(pos[None, :], (32, S)))
    d["posrow"] = np.ascontiguousarray(pos[None, :])
    d["refrow"] = np.ascontiguousarray(np.repeat(pos[0::128], 128)[None, :])
    return d


def prepA_layer_head(inp, l, h):
    w_in = inp["w_in"][l]
    cols = np.zeros(NT, dtype=np.int64)

    def put(name, idx):
        off, m = G[name]
        assert len(idx) == m
        cols[off:off + m] = idx

    put("cq0", np.arange(0, 128))
    put("cq1", np.arange(128, 256))
    put("ckv", np.arange(256, 384))
    kr = np.arange(384, 416)
    krs = np.concatenate([kr[16:], kr[:16]])
    put("kr", np.concatenate([np.arange(0, 64), kr]))
    put("krs", np.concatenate([np.arange(0, 64), krs]))
    sb0 = 416
    sbq = sb0 + h * 64 + np.arange(64)
    sbk = sb0 + 256 + h * 64 + np.arange(64)
    sbv = sb0 + 512 + h * 64 + np.arange(64)
    swq0 = 1184
    swqA = swq0 + (2 * h) * 64 + np.arange(64)
    swqB = swq0 + (2 * h + 1) * 64 + np.arange(64)
    swk = 1696 + (h // 2) * 64 + np.arange(64)
    swv = 1696 + 128 + (h // 2) * 64 + np.arange(64)
    dfq1 = 1952 + h * 64 + np.arange(32)
    dfq2 = 1952 + h * 64 + 32 + np.arange(32)
    dfk1 = 2208 + h * 64 + np.arange(32)
    dfk2 = 2208 + h * 64 + 32 + np.arange(32)
    dfv = 2464 + h * 64 + np.arange(64)
    put("qB", np.concatenate([sbq, dfq1]))
    put("kB", np.concatenate([sbk, dfk1]))
    put("qC", dfq2)
    put("kC", dfk2)
    put("swqA", swqA)
    put("swqB", swqB)
    put("swk", swk)
    d = {}
    d["wT"] = np.ascontiguousarray(w_in[:, cols])
    d["wV"] = np.ascontiguousarray(w_in[:, np.concatenate([sbv, swv, dfv])])
    wq = inp["w_uq"][l][:, h * 96:(h + 1) * 96]
    perm = np.concatenate([np.arange(64), 64 + 16 + np.arange(16), 64 + np.arange(16)])
    d["wuq"] = np.ascontiguousarray(np.concatenate([wq, wq[:, perm]], axis=1))
    d["wukv"] = np.ascontiguousarray(inp["w_ukv"][l][:, h * 128:(h + 1) * 128])
    d["qn"] = np.ascontiguousarray(inp["mla_q_norm"][l].reshape(2, 128).T)
    d["kvn"] = np.ascontiguousarray(inp["mla_kv_norm"][l].reshape(128, 1))
    cst = np.zeros((128, 16), np.float32)
    half = 16
    inv_freq = (np.float32(10000.0) ** (-np.arange(half, dtype=np.float32) / np.float32(half))).astype(np.float32)
    cst[64:96, 0] = np.concatenate([inv_freq, inv_freq])
    cst[64:96, 1] = np.concatenate([-np.ones(16), np.ones(16)])
    cst[:, 2] = alibi(4)[h]
    cst[:, 3] = alibi(8)[2 * h]
    cst[:, 4] = alibi(8)[2 * h + 1]
    lam_init = 0.8 - 0.6 * math.exp(-0.3 * l)
    cst[:, 7] = lam_init
    cst[:, 8] = 1.0 - lam_init
    cst[:, 9] = inp["sw_sinks"][l][2 * h]
    cst[:, 10] = inp["sw_sinks"][l][2 * h + 1]
    cst[:, 11] = math.pi / 2
    cst[:, 12] = RMS_EPS
    cst[0:64, 13] = inp["df_subln"][l]
    d["cst"] = cst
    lqk = np.concatenate([inp["df_lq1"][l], inp["df_lk1"][l], inp["df_lq2"][l], inp["df_lk2"][l]])
    d["lqk"] = np.ascontiguousarray(np.broadcast_to(lqk[None, :], (64, 128))).astype(np.float32)
    return d

def prepB1_weights(inp, l):
    d = {}
    d["w_gate"] = np.ascontiguousarray(inp["w_gate"][l])
    d["bgT"] = np.ascontiguousarray(inp["b_gate"][l].reshape(4, 8, 128).transpose(2, 0, 1).reshape(128, 32))
    d["w_br"] = np.ascontiguousarray(np.concatenate([inp["w_br_a"][l], inp["w_br_b"][l], inp["w_br_c"][l], inp["w_br_d"][l]], axis=0))
    d["w_out"] = np.ascontiguousarray(inp["w_out"][l])
    d["lng"] = np.ascontiguousarray(np.broadcast_to(inp["ln1_g"][l][None, :], (128, 1024)))
    d["lnb"] = np.ascontiguousarray(np.broadcast_to(inp["ln1_b"][l][None, :], (128, 1024)))
    d["w_router"] = np.ascontiguousarray(inp["w_router"])
    d["rbias"] = np.ascontiguousarray(np.broadcast_to(inp["router_bias"][None, :], (128, 32)))
    return d

def prepB2_weights(inp, l):
    d = {}
    d["w1"] = np.ascontiguousarray(inp["moe_w1"][l])
    d["w3"] = np.ascontiguousarray(inp["moe_w3"][l])
    d["w2"] = np.ascontiguousarray(inp["moe_w2"][l])
    d["lng"] = np.ascontiguousarray(np.broadcast_to(inp["ln2_g"][l][None, :], (128, 1024)))
    d["lnb"] = np.ascontiguousarray(np.broadcast_to(inp["ln2_b"][l][None, :], (128, 1024)))
    return d

def assemble_oT(resA, nb=2, S=8192):
    out = np.zeros((1280, nb * S), dtype=resA[0]["oT"].dtype)
    for b in range(nb):
        for h in range(4):
            o = resA[b * 4 + h]["oT"]
            cs = slice(b * S, (b + 1) * S)
            out[h * 64:(h + 1) * 64, cs] = o[0:64]
            out[256 + h * 64:256 + (h + 1) * 64, cs] = o[64:128]
            out[512 + 2 * h * 64:512 + (2 * h + 1) * 64, cs] = o[128:192]
            out[512 + (2 * h + 1) * 64:512 + (2 * h + 2) * 64, cs] = o[192:256]
            out[1024 + h * 64:1024 + (h + 1) * 64, cs] = o[256:320]
    return out


_CACHE = {}


def _progs():
    if "A" not in _CACHE:
        _CACHE["A"] = build_phaseA(8192)
        _CACHE["B1"] = build_B1()
        _CACHE["B2"] = build_B2()
    return _CACHE["A"], _CACHE["B1"], _CACHE["B2"]


def kernel(**inp):
    inp = {k: np.asarray(v) for k, v in inp.items()}
    S = 8192
    ncA, ncB1, ncB2 = _progs()
    shared = prepA_shared(inp, S)
    xcur = np.ascontiguousarray(inp["x"].astype(np.float32).reshape(2 * S, 1024))
    cores = list(range(8))
    for l in range(4):
        maps = []
        for b in range(2):
            xT = np.ascontiguousarray(xcur[b * S:(b + 1) * S].T)
            for h in range(4):
                m = dict(shared)
                m.update(prepA_layer_head(inp, l, h))
                m["xT"] = xT
                maps.append(m)
        resA = run_bass_kernel_spmd(ncA, maps, core_ids=cores).results
        oT = assemble_oT([{"oT": np.asarray(r["oT"])} for r in resA])
        w = prepB1_weights(inp, l)
        maps = []
        for c in range(8):
            sl = slice(c * TOK, (c + 1) * TOK)
            m = dict(w)
            m["xT"] = np.ascontiguousarray(xcur[sl].T)
            m["x"] = np.ascontiguousarray(xcur[sl])
            m["oT"] = np.ascontiguousarray(oT[:, sl])
            maps.append(m)
        res1 = run_bass_kernel_spmd(ncB1, maps, core_ids=cores).results
        w = prepB2_weights(inp, l)
        maps = []
        for c in range(8):
            m = dict(w)
            for k in ("y1", "x2T", "gw"):
                m[k] = np.asarray(res1[c][k])
            maps.append(m)
        res2 = run_bass_kernel_spmd(ncB2, maps, core_ids=cores).results
        xcur = np.ascontiguousarray(np.concatenate([np.asarray(res2[c]["xo"]) for c in range(8)], axis=0))
    return xcur.reshape(2, S, 1024).astype(np.float32)
```
